# Optimizing a Trainium2 kernel written in Bass

```python
import math
import jax
import jax.numpy as jnp
from jax import lax
import numpy as np

D_MODEL = 2048
BATCH = 16
SEQ = 2048
DEPTH = 2

GRID_W = 64
CTX_LEN = 256
HEAD_DIM = 128
MIX_HEADS = D_MODEL // HEAD_DIM
ROPE_THETA = 10000.0
SCALE = HEAD_DIM ** -0.5
NEG_INF = -1e30
A_HEADS = MIX_HEADS // 2
A_KV_HEADS = A_HEADS // 4
A_WINDOW = 128
A_BLOCK = 128
B_HEADS = MIX_HEADS - A_HEADS
NA_ROWS = 8
NA_COLS = 16
C_HEADS = MIX_HEADS // 2
C_KV_HEADS = C_HEADS // 4
D_HEADS = (MIX_HEADS - C_HEADS) // 2
Q_BLOCK = 128
EVEN_SPLITS = (A_HEADS * HEAD_DIM, A_KV_HEADS * HEAD_DIM, A_KV_HEADS * HEAD_DIM,
               B_HEADS * HEAD_DIM, B_HEADS * HEAD_DIM, B_HEADS * HEAD_DIM)
ODD_SPLITS = (C_HEADS * HEAD_DIM, C_KV_HEADS * HEAD_DIM, C_KV_HEADS * HEAD_DIM,
              D_HEADS * 2 * HEAD_DIM, D_HEADS * 2 * HEAD_DIM, D_HEADS * 2 * HEAD_DIM)
EVEN_OUT = (A_HEADS + B_HEADS) * HEAD_DIM
ODD_OUT = (C_HEADS + 2 * D_HEADS) * HEAD_DIM
N_EXPERTS = 16
N_GROUPS = 4
EXPERTS_PER_GROUP = N_EXPERTS // N_GROUPS
TOP_K = 2
EXPERT_FF = D_MODEL // 2
ALPHA = (2 * DEPTH) ** 0.25
BETA = (8 * DEPTH) ** -0.25

kernel_name = 'hybrid_diffusion_prefix_block'


def _cuts(sizes):
    return [int(v) for v in np.cumsum(sizes)[:-1]]


def layer_norm(x, g, b, eps=1e-5):
    xf = x.astype(jnp.float32)
    mu = jnp.mean(xf, -1, keepdims=True)
    var = jnp.mean(jnp.square(xf - mu), -1, keepdims=True)
    return ((xf - mu) * lax.rsqrt(var + eps)).astype(x.dtype) * g + b


def rms_norm(x, g, eps=1e-6):
    xf = x.astype(jnp.float32)
    return (xf * lax.rsqrt(jnp.mean(jnp.square(xf), -1, keepdims=True) + eps)).astype(x.dtype) * g


def axial_rope(n_tokens):
    t = jnp.arange(n_tokens, dtype=jnp.int32)
    row = (t // GRID_W).astype(jnp.float32)
    col = (t % GRID_W).astype(jnp.float32)
    n_freq = HEAD_DIM // 4
    inv = ROPE_THETA ** (-jnp.arange(n_freq, dtype=jnp.float32) / n_freq)
    ang = jnp.concatenate([row[:, None] * inv[None], col[:, None] * inv[None]], -1)
    return jnp.cos(ang), jnp.sin(ang)


def apply_rope(x, cos, sin):
    shp = (1, x.shape[1]) + (1,) * (x.ndim - 3) + (cos.shape[-1],)
    c = cos.reshape(shp).astype(x.dtype)
    s = sin.reshape(shp).astype(x.dtype)
    x1, x2 = x[..., 0::2], x[..., 1::2]
    return jnp.stack([x1 * c - x2 * s, x1 * s + x2 * c], -1).reshape(x.shape)


def softmax_with_sink(s, sink):
    sk = jnp.broadcast_to(sink.astype(jnp.float32)[None, :, :, None, None], s.shape[:-1] + (1,))
    return jax.nn.softmax(jnp.concatenate([s, sk], -1), -1)[..., :-1]


def ctx_gqa(q, k, v, sink=None):
    B, L = q.shape[:2]
    s = jnp.einsum('bqhgd,bkhd->bhgqk', q, k, preferred_element_type=jnp.float32) * SCALE
    p = jax.nn.softmax(s, -1) if sink is None else softmax_with_sink(s, sink)
    o = jnp.einsum('bhgqk,bkhd->bqhgd', p.astype(v.dtype), v)
    return o.reshape(B, L, -1)


def windowed_gqa_sink(q, q_nopos, k, v, k_ctx, v_ctx, sink):
    B, S, KV, G, Dh = q.shape
    nblk = S // A_BLOCK
    span = 3 * A_BLOCK
    pad = jnp.zeros((B, A_BLOCK, KV, Dh), k.dtype)
    k_pad = jnp.concatenate([pad, k, pad], 1)
    v_pad = jnp.concatenate([pad, v, pad], 1)
    rel = np.arange(span)[None, :] - np.arange(A_BLOCK)[:, None]
    band = jnp.asarray((rel >= A_BLOCK - A_WINDOW) & (rel <= A_BLOCK + A_WINDOW))
    to_blocks = lambda t: jnp.moveaxis(t.reshape(B, nblk, A_BLOCK, KV, G, Dh), 1, 0)

    def block(args):
        b, qb, qnb = args
        kb = lax.dynamic_slice_in_dim(k_pad, b * A_BLOCK, span, axis=1)
        vb = lax.dynamic_slice_in_dim(v_pad, b * A_BLOCK, span, axis=1)
        kpos = b * A_BLOCK - A_BLOCK + jnp.arange(span)
        valid = band & ((kpos >= 0) & (kpos < S))[None, :]
        s_loc = jnp.einsum('bqhgd,bkhd->bhgqk', qb, kb, preferred_element_type=jnp.float32) * SCALE
        s_loc = jnp.where(valid, s_loc, NEG_INF)
        s_ctx = jnp.einsum('bqhgd,bkhd->bhgqk', qnb, k_ctx, preferred_element_type=jnp.float32) * SCALE
        p = softmax_with_sink(jnp.concatenate([s_loc, s_ctx], -1), sink).astype(v.dtype)
        o = (jnp.einsum('bhgqk,bkhd->bqhgd', p[..., :span], vb)
             + jnp.einsum('bhgqk,bkhd->bqhgd', p[..., span:], v_ctx))
        return o.reshape(B, A_BLOCK, KV * G * Dh)

    o = lax.map(block, (jnp.arange(nblk), to_blocks(q), to_blocks(q_nopos)))
    return jnp.moveaxis(o, 0, 1).reshape(B, S, KV * G * Dh)


def neighbourhood_attn(q, k, v, k_ctx, v_ctx, rpb):
    B, S, H, Dh = q.shape
    rows = S // GRID_W
    kh, kw = min(NA_ROWS, rows), NA_COLS
    q_rows = jnp.moveaxis(q.reshape(B, rows, GRID_W, H, Dh), 1, 0)
    k_grid = k.reshape(B, rows, GRID_W, H, Dh)
    v_grid = v.reshape(B, rows, GRID_W, H, Dh)
    col = np.arange(GRID_W)
    col_start = np.clip(col - kw // 2, 0, GRID_W - kw)
    col_ok = jnp.asarray((col[None, :] >= col_start[:, None]) & (col[None, :] < col_start[:, None] + kw))
    dcol = jnp.asarray(np.clip(col[None, :] - col[:, None] + NA_COLS - 1, 0, 2 * NA_COLS - 2))
    n_loc = kh * GRID_W

    def row_block(args):
        r, qr = args
        r0 = jnp.clip(r - kh // 2, 0, rows - kh)
        kr = lax.dynamic_slice_in_dim(k_grid, r0, kh, axis=1)
        vr = lax.dynamic_slice_in_dim(v_grid, r0, kh, axis=1)
        drow = r0 + jnp.arange(kh) - r + NA_ROWS - 1
        bias = rpb[:, drow[None, :, None], dcol[:, None, :]].astype(jnp.float32)
        s = jnp.einsum('bqhd,bijhd->bhqij', qr, kr, preferred_element_type=jnp.float32) * SCALE + bias[None]
        s = jnp.where(col_ok[:, None, :], s, NEG_INF).reshape(B, H, GRID_W, n_loc)
        s_ctx = jnp.einsum('bqhd,bkhd->bhqk', qr, k_ctx, preferred_element_type=jnp.float32) * SCALE
        p = jax.nn.softmax(jnp.concatenate([s, s_ctx], -1), -1).astype(v.dtype)
        o = (jnp.einsum('bhqn,bnhd->bqhd', p[..., :n_loc], vr.reshape(B, n_loc, H, Dh))
             + jnp.einsum('bhqk,bkhd->bqhd', p[..., n_loc:], v_ctx))
        return o.reshape(B, GRID_W, H * Dh)

    o = lax.map(row_block, (jnp.arange(rows), q_rows))
    return jnp.moveaxis(o, 0, 1).reshape(B, S, H * Dh)


def dense_gqa_blocks(q, q_nopos, k, v, k_ctx, v_ctx):
    B, S, KV, G, Dh = q.shape
    nblk = S // Q_BLOCK
    to_blocks = lambda t: jnp.moveaxis(t.reshape(B, nblk, Q_BLOCK, KV, G, Dh), 1, 0)

    def block(args):
        qb, qnb = args
        s = jnp.einsum('bqhgd,bkhd->bhgqk', qb, k, preferred_element_type=jnp.float32) * SCALE
        s_ctx = jnp.einsum('bqhgd,bkhd->bhgqk', qnb, k_ctx, preferred_element_type=jnp.float32) * SCALE
        p = jax.nn.softmax(jnp.concatenate([s, s_ctx], -1), -1).astype(v.dtype)
        o = (jnp.einsum('bhgqk,bkhd->bqhgd', p[..., :S], v)
             + jnp.einsum('bhgqk,bkhd->bqhgd', p[..., S:], v_ctx))
        return o.reshape(B, Q_BLOCK, KV * G * Dh)

    o = lax.map(block, (to_blocks(q), to_blocks(q_nopos)))
    return jnp.moveaxis(o, 0, 1).reshape(B, S, KV * G * Dh)


def diff_attn_blocks(q, q_nopos, k, v, k_ctx, v_ctx, lam):
    B, S, H, _, Dh = q.shape
    nblk = S // Q_BLOCK
    to_blocks = lambda t: jnp.moveaxis(t.reshape(B, nblk, Q_BLOCK, H, 2, Dh), 1, 0)

    def block(args):
        qb, qnb = args
        s = jnp.einsum('bqhtd,bkhtd->bhtqk', qb, k, preferred_element_type=jnp.float32) * SCALE
        s_ctx = jnp.einsum('bqhtd,bkhtd->bhtqk', qnb, k_ctx, preferred_element_type=jnp.float32) * SCALE
        p = jax.nn.softmax(jnp.concatenate([s, s_ctx], -1), -1)
        a = (p[:, :, 0] - lam * p[:, :, 1]).astype(v.dtype)
        return (jnp.einsum('bhqk,bkhe->bqhe', a[..., :S], v)
                + jnp.einsum('bhqk,bkhe->bqhe', a[..., S:], v_ctx))

    o = lax.map(block, (to_blocks(q), to_blocks(q_nopos)))
    return jnp.moveaxis(o, 0, 1).reshape(B, S, H, 2 * Dh)


def ctx_diff(q, k, v, lam):
    s = jnp.einsum('bqhtd,bkhtd->bhtqk', q, k, preferred_element_type=jnp.float32) * SCALE
    p = jax.nn.softmax(s, -1)
    a = (p[:, :, 0] - lam * p[:, :, 1]).astype(v.dtype)
    return jnp.einsum('bhqk,bkhe->bqhe', a, v)


def even_mixer(h, hc, w_in, w_out, sink, rpb, cos, sin, ctx_out):
    B, S, _ = h.shape
    L = hc.shape[1]
    G = A_HEADS // A_KV_HEADS
    cuts = _cuts(EVEN_SPLITS)
    a_q, a_k, a_v, b_q, b_k, b_v = jnp.split(h @ w_in, cuts, axis=-1)
    a_qx, a_kx, a_vx, b_qx, b_kx, b_vx = jnp.split(hc @ w_in, cuts, axis=-1)
    sink = sink.reshape(A_KV_HEADS, G)
    a_q = a_q.reshape(B, S, A_KV_HEADS, G, HEAD_DIM)
    a_k = a_k.reshape(B, S, A_KV_HEADS, HEAD_DIM)
    a_v = a_v.reshape(B, S, A_KV_HEADS, HEAD_DIM)
    a_kx = a_kx.reshape(B, L, A_KV_HEADS, HEAD_DIM)
    a_vx = a_vx.reshape(B, L, A_KV_HEADS, HEAD_DIM)
    o_a = windowed_gqa_sink(apply_rope(a_q, cos, sin), a_q, apply_rope(a_k, cos, sin), a_v, a_kx, a_vx, sink)
    b_kx = b_kx.reshape(B, L, B_HEADS, HEAD_DIM)
    b_vx = b_vx.reshape(B, L, B_HEADS, HEAD_DIM)
    o_b = neighbourhood_attn(b_q.reshape(B, S, B_HEADS, HEAD_DIM), b_k.reshape(B, S, B_HEADS, HEAD_DIM),
                             b_v.reshape(B, S, B_HEADS, HEAD_DIM), b_kx, b_vx, rpb)
    y = jnp.concatenate([o_a, o_b], -1) @ w_out
    if not ctx_out:
        return y, None
    oc_a = ctx_gqa(a_qx.reshape(B, L, A_KV_HEADS, G, HEAD_DIM), a_kx, a_vx, sink)
    oc_b = ctx_gqa(b_qx.reshape(B, L, B_HEADS, 1, HEAD_DIM), b_kx, b_vx)
    yc = jnp.concatenate([oc_a, oc_b], -1) @ w_out
    return y, yc


def odd_mixer(h, hc, w_in, w_out, q_norm_g, k_norm_g, lambda_q1, lambda_k1, lambda_q2, lambda_k2,
              subln_g, lambda_init, cos, sin, ctx_out):
    B, S, _ = h.shape
    L = hc.shape[1]
    G = C_HEADS // C_KV_HEADS
    cuts = _cuts(ODD_SPLITS)
    c_q, c_k, c_v, d_q, d_k, d_v = jnp.split(h @ w_in, cuts, axis=-1)
    c_qx, c_kx, c_vx, d_qx, d_kx, d_vx = jnp.split(hc @ w_in, cuts, axis=-1)
    q = rms_norm(c_q.reshape(B, S, C_KV_HEADS, G, HEAD_DIM), q_norm_g)
    k = rms_norm(c_k.reshape(B, S, C_KV_HEADS, HEAD_DIM), k_norm_g)
    v = c_v.reshape(B, S, C_KV_HEADS, HEAD_DIM)
    k_x = rms_norm(c_kx.reshape(B, L, C_KV_HEADS, HEAD_DIM), k_norm_g)
    v_x = c_vx.reshape(B, L, C_KV_HEADS, HEAD_DIM)
    o_c = dense_gqa_blocks(apply_rope(q, cos, sin), q, apply_rope(k, cos, sin), v, k_x, v_x)
    lam = (jnp.exp(jnp.sum(lambda_q1.astype(jnp.float32) * lambda_k1.astype(jnp.float32)))
           - jnp.exp(jnp.sum(lambda_q2.astype(jnp.float32) * lambda_k2.astype(jnp.float32))) + lambda_init)
    dq = d_q.reshape(B, S, D_HEADS, 2, HEAD_DIM)
    dk = d_k.reshape(B, S, D_HEADS, 2, HEAD_DIM)
    dv = d_v.reshape(B, S, D_HEADS, 2 * HEAD_DIM)
    dk_x = d_kx.reshape(B, L, D_HEADS, 2, HEAD_DIM)
    dv_x = d_vx.reshape(B, L, D_HEADS, 2 * HEAD_DIM)
    o_d = diff_attn_blocks(apply_rope(dq, cos, sin), dq, apply_rope(dk, cos, sin), dv, dk_x, dv_x, lam)
    o_d = rms_norm(o_d, subln_g) * (1.0 - lambda_init)
    y = jnp.concatenate([o_c, o_d.reshape(B, S, -1)], -1) @ w_out
    if not ctx_out:
        return y, None
    q_x = rms_norm(c_qx.reshape(B, L, C_KV_HEADS, G, HEAD_DIM), q_norm_g)
    oc_c = ctx_gqa(q_x, k_x, v_x)
    oc_d = rms_norm(ctx_diff(d_qx.reshape(B, L, D_HEADS, 2, HEAD_DIM), dk_x, dv_x, lam), subln_g) * (1.0 - lambda_init)
    yc = jnp.concatenate([oc_c, oc_d.reshape(B, L, -1)], -1) @ w_out
    return y, yc


def moe(h, w_router, router_bias, w_gate, w_up, w_down):
    logits = jnp.einsum('...d,de->...e', h, w_router, preferred_element_type=jnp.float32)
    scores = jax.nn.sigmoid(logits)
    biased = scores + router_bias.astype(jnp.float32)
    grouped = biased.reshape(biased.shape[:-1] + (N_GROUPS, EXPERTS_PER_GROUP))
    group_score = lax.top_k(grouped, TOP_K)[0].sum(-1)
    best = jnp.argmax(group_score, -1)
    in_group = best[..., None] == jnp.arange(N_GROUPS)
    masked = jnp.where(in_group[..., None], grouped, -jnp.inf).reshape(biased.shape)
    _, idx = lax.top_k(masked, TOP_K)
    w = jnp.take_along_axis(scores, idx, -1)
    w = w / jnp.sum(w, -1, keepdims=True)
    gates = jnp.sum(jax.nn.one_hot(idx, N_EXPERTS, dtype=jnp.float32) * w[..., None], -2).astype(h.dtype)
    out = jnp.zeros_like(h)
    for e in range(N_EXPERTS):
        a = jax.nn.silu(h @ w_gate[e]) * (h @ w_up[e])
        out = out + gates[..., e:e + 1] * (a @ w_down[e])
    return out


def setup_inputs(seed: int = 0) -> dict:
    key = jax.random.key(seed)
    ks = jax.random.split(key, 28)

    def nrm(i, shape, scale):
        return jax.random.normal(ks[i], shape, jnp.float32) * scale

    n_even = (DEPTH + 1) // 2
    n_odd = DEPTH // 2
    return {
        'x': nrm(0, (BATCH, SEQ, D_MODEL), 1.0),
        'c': nrm(1, (BATCH, D_MODEL), 1.0),
        'ctx': nrm(2, (BATCH, CTX_LEN, D_MODEL), 1.0),
        'c_ctx': nrm(3, (D_MODEL,), 1.0),
        'w_ada': nrm(4, (DEPTH, D_MODEL, 6 * D_MODEL), 0.5 * D_MODEL ** -0.5),
        'b_ada': nrm(5, (DEPTH, 6 * D_MODEL), 0.02),
        'ln1_g': 1.0 + nrm(6, (DEPTH, D_MODEL), 0.02),
        'ln1_b': nrm(7, (DEPTH, D_MODEL), 0.02),
        'ln2_g': 1.0 + nrm(8, (DEPTH, D_MODEL), 0.02),
        'ln2_b': nrm(9, (DEPTH, D_MODEL), 0.02),
        'w_in_even': nrm(10, (n_even, D_MODEL, sum(EVEN_SPLITS)), D_MODEL ** -0.5),
        'w_out_even': nrm(11, (n_even, EVEN_OUT, D_MODEL), BETA * EVEN_OUT ** -0.5),
        'sink_logits': nrm(12, (n_even, A_HEADS), 0.5),
        'na_rpb': nrm(13, (n_even, B_HEADS, 2 * NA_ROWS - 1, 2 * NA_COLS - 1), 0.1),
        'w_in_odd': nrm(14, (n_odd, D_MODEL, sum(ODD_SPLITS)), D_MODEL ** -0.5),
        'w_out_odd': nrm(15, (n_odd, ODD_OUT, D_MODEL), BETA * ODD_OUT ** -0.5),
        'q_norm_g': 1.0 + nrm(16, (n_odd, HEAD_DIM), 0.02),
        'k_norm_g': 1.0 + nrm(17, (n_odd, HEAD_DIM), 0.02),
        'lambda_q1': nrm(18, (n_odd, HEAD_DIM), 0.1),
        'lambda_k1': nrm(19, (n_odd, HEAD_DIM), 0.1),
        'lambda_q2': nrm(20, (n_odd, HEAD_DIM), 0.1),
        'lambda_k2': nrm(21, (n_odd, HEAD_DIM), 0.1),
        'subln_g': 1.0 + nrm(22, (n_odd, 2 * HEAD_DIM), 0.02),
        'w_router': nrm(23, (D_MODEL, N_EXPERTS), D_MODEL ** -0.5),
        'router_bias': nrm(24, (N_EXPERTS,), 0.01),
        'w_exp_gate': nrm(25, (DEPTH, N_EXPERTS, D_MODEL, EXPERT_FF), D_MODEL ** -0.5),
        'w_exp_up': nrm(26, (DEPTH, N_EXPERTS, D_MODEL, EXPERT_FF), D_MODEL ** -0.5),
        'w_exp_down': nrm(27, (DEPTH, N_EXPERTS, EXPERT_FF, D_MODEL), BETA * EXPERT_FF ** -0.5),
    }


def reference(x, c, ctx, c_ctx, w_ada, b_ada, ln1_g, ln1_b, ln2_g, ln2_b,
              w_in_even, w_out_even, sink_logits, na_rpb,
              w_in_odd, w_out_odd, q_norm_g, k_norm_g,
              lambda_q1, lambda_k1, lambda_q2, lambda_k2, subln_g,
              w_router, router_bias, w_exp_gate, w_exp_up, w_exp_down):
    S = x.shape[1]
    cos, sin = axial_rope(S)
    silu_c = jax.nn.silu(c)
    silu_cc = jax.nn.silu(c_ctx)
    for l in range(DEPTH):
        last = l == DEPTH - 1
        j = l // 2
        mod = silu_c @ w_ada[l] + b_ada[l]
        sh1, sc1, g1, sh2, sc2, g2 = [m[:, None, :] for m in jnp.split(mod, 6, axis=-1)]
        modc = silu_cc @ w_ada[l] + b_ada[l]
        csh1, csc1, cg1, csh2, csc2, cg2 = jnp.split(modc, 6)
        h = x * (1.0 + sc1) + sh1
        hc = ctx * (1.0 + csc1) + csh1
        if l % 2 == 0:
            y, yc = even_mixer(h, hc, w_in_even[j], w_out_even[j], sink_logits[j], na_rpb[j], cos, sin, not last)
        else:
            lambda_init = 0.8 - 0.6 * math.exp(-0.3 * l)
            y, yc = odd_mixer(h, hc, w_in_odd[j], w_out_odd[j], q_norm_g[j], k_norm_g[j],
                              lambda_q1[j], lambda_k1[j], lambda_q2[j], lambda_k2[j], subln_g[j],
                              lambda_init, cos, sin, not last)
        x = layer_norm(ALPHA * x + g1 * y, ln1_g[l], ln1_b[l])
        f = moe(x * (1.0 + sc2) + sh2, w_router, router_bias, w_exp_gate[l], w_exp_up[l], w_exp_down[l])
        x = layer_norm(ALPHA * x + g2 * f, ln2_g[l], ln2_b[l])
        if not last:
            ctx = layer_norm(ALPHA * ctx + cg1 * yc, ln1_g[l], ln1_b[l])
            fc = moe(ctx * (1.0 + csc2) + csh2, w_router, router_bias, w_exp_gate[l], w_exp_up[l], w_exp_down[l])
            ctx = layer_norm(ALPHA * ctx + cg2 * fc, ln2_g[l], ln2_b[l])
    return x
```

```python
import math
import contextlib
import numpy as np
import concourse.bass as bass
import concourse.mybir as mybir
from concourse.bass_utils import run_bass_kernel_spmd

F32 = mybir.dt.float32
BF16 = mybir.dt.bfloat16
AF = mybir.ActivationFunctionType
ALU = mybir.AluOpType
AX = mybir.AxisListType

NCORES = 8
D = 2048
NTOK = 4608
NLAT = 4096
ALPHA = 4.0 ** 0.25
LN_EPS = 1e-5 / (ALPHA * ALPHA)
SCALE = 128.0 ** -0.5
LAMBDA_INIT1 = 0.8 - 0.6 * math.exp(-0.3 * 1)
BIG = 1.0e4


class Buf:
    __slots__ = ("w", "r", "dkey", "epoch")

    def __init__(self):
        self.w = None
        self.r = {}
        self.dkey = None
        self.epoch = -1


class Sched:
    ENGS = ("pe", "act", "dve", "pool", "sp")

    def __init__(self, nc):
        self.nc = nc
        self.q = {e: [] for e in self.ENGS}
        self.cnt = {}
        self.seen = {e: {} for e in self.ENGS}
        self.epoch = 0
        self.free = []
        self.nd = 0

    def _dkey(self, b):
        if b.dkey is None or b.epoch != self.epoch:
            if self.free:
                b.dkey = self.free.pop()
            else:
                b.dkey = ("d", self.nd)
                self.nd += 1
            b.epoch = self.epoch
            self.live.append(b.dkey)
        return b.dkey

    live = None

    def op(self, eng, fn, reads=(), writes=(), dma=False, part=False):
        if self.live is None:
            self.live = []
        waits = {}
        seen = self.seen[eng]

        def need(tok):
            if tok is None:
                return
            k, v = tok
            if k == "pe" and eng == "pe":
                return
            if seen.get(k, 0) >= v:
                return
            if waits.get(k, 0) < v:
                waits[k] = v

        for b in reads:
            need(b.w)
        for i, b in enumerate(writes):
            if not (part and i == 0):
                need(b.w)
            for k, v in b.r.items():
                need((k, v))
        for k, v in waits.items():
            seen[k] = v
        if dma:
            key, amt = self._dkey(writes[0]), 16
        else:
            key, amt = eng, 1
        val = self.cnt.get(key, 0) + amt
        self.cnt[key] = val
        tok = (key, val)
        for b in reads:
            if b.r.get(key, 0) < val:
                b.r[key] = val
        for b in writes:
            b.w = tok
            b.r = {}
        self.q[eng].append((tuple(waits.items()), fn, key, amt))
        return tok

    def barrier(self):
        for e in self.ENGS:
            seen = self.seen[e]
            waits = {}
            for k, v in self.cnt.items():
                if seen.get(k, 0) < v and not (k == e and e == "pe"):
                    waits[k] = v
                    seen[k] = v
            self.q[e].append((tuple(waits.items()), None, None, 0))
        if self.live:
            self.free.extend(self.live)
            self.live = []
        self.epoch += 1

    def emit(self):
        nc = self.nc
        sems = {}
        with contextlib.ExitStack() as st:
            for k in self.cnt.keys():
                nm = "s_" + (k if isinstance(k, str) else "d%d" % k[1])
                sems[k] = st.enter_context(nc.semaphore(nm))
            block = st.enter_context(nc.Block())
            engmap = {"pe": block.tensor, "act": block.scalar, "dve": block.vector,
                      "pool": block.gpsimd, "sp": block.sync}
            for en in self.ENGS:
                lst = self.q[en]

                def body(e, lst=lst):
                    for waits, fn, key, amt in lst:
                        for k, v in waits:
                            e.wait_ge(sems[k], v)
                        if fn is not None:
                            fn(e).then_inc(sems[key], amt)
                engmap[en](body)


class KB:
    def __init__(self, S):
        self.S = S

    def dma(self, q, out, in_, reads, writes, part=False):
        return self.S.op(q, lambda e: e.dma_start(out=out, in_=in_), reads, writes, dma=True, part=part)

    def mm(self, out, lhsT, rhs, start, stop, reads, writes, part=False):
        return self.S.op("pe", lambda e: e.matmul(out, lhsT=lhsT, rhs=rhs, start=start, stop=stop), reads, writes, part=part)

    def tr(self, out, in_, ident, reads, writes, part=False):
        return self.S.op("pe", lambda e: e.transpose(out, in_, ident), reads, writes, part=part)

    def act(self, out, in_, func, reads, writes, bias=None, scale=None, part=False):
        kw = {}
        if bias is not None:
            kw["bias"] = bias
        if scale is not None:
            kw["scale"] = scale
        return self.S.op("act", lambda e: e.activation(out=out, in_=in_, func=func, **kw), reads, writes, part=part)

    def ts(self, out, in0, s1, s2, op0, op1, reads, writes, eng="dve", part=False):
        if op1 is None:
            return self.S.op(eng, lambda e: e.tensor_scalar(out=out, in0=in0, scalar1=s1, scalar2=None, op0=op0), reads, writes, part=part)
        return self.S.op(eng, lambda e: e.tensor_scalar(out=out, in0=in0, scalar1=s1, scalar2=s2, op0=op0, op1=op1), reads, writes, part=part)

    def tt(self, out, in0, in1, op, reads, writes, eng="dve", part=False):
        return self.S.op(eng, lambda e: e.tensor_tensor(out=out, in0=in0, in1=in1, op=op), reads, writes, part=part)

    def stt(self, out, in0, scalar, in1, op0, op1, reads, writes, eng="dve", part=False):
        return self.S.op(eng, lambda e: e.scalar_tensor_tensor(out=out, in0=in0, scalar=scalar, in1=in1, op0=op0, op1=op1), reads, writes, part=part)

    def cp(self, out, in_, reads, writes, eng="dve", part=False):
        return self.S.op(eng, lambda e: e.tensor_copy(out=out, in_=in_), reads, writes, part=part)

    def red(self, out, in_, op, reads, writes, part=False):
        return self.S.op("dve", lambda e: e.tensor_reduce(out=out, in_=in_, axis=AX.X, op=op), reads, writes, part=part)

    def recip(self, out, in_, reads, writes, part=False):
        return self.S.op("dve", lambda e: e.reciprocal(out=out, in_=in_), reads, writes, part=part)

    def rsqrt(self, out, in_, eps, reads, writes):
        self.ts(out, in_, eps, None, ALU.add, None, reads, writes)
        self.act(out, out, AF.Sqrt, list(writes), writes)
        return self.recip(out, out, list(writes), writes)

    def memset(self, out, val, writes, eng="dve"):
        return self.S.op(eng, lambda e: e.memset(out, val), (), writes)


def build(dbg=False, stop=99):
    nc = bass.Bass("TRN2", target_bir_lowering=False)
    S = Sched(nc)
    S.live = []
    K = KB(S)

    def din(name, shape, dt=F32):
        return nc.dram_tensor(name, list(shape), dt, kind="ExternalInput").ap()
    skind = "ExternalOutput" if dbg else "Internal"

    def dscr(name, shape, dt):
        return nc.dram_tensor(name, list(shape), dt, kind=skind).ap()

    xin = din("xin", [D, NTOK]); cin = din("cin", [128, 16, 3]); w_ada = din("w_ada", [2, D, 6 * D])
    bada = din("bada", [128, 2, 96]); lnp = din("lnp", [128, 2, 4, 16])
    w_in = [din("w_in0", [D, 4608]), din("w_in1", [D, 4608])]
    w_out = [din("w_out0", [D, D]), din("w_out1", [D, D])]
    sinkb = din("sinkb", [128, 8]); rpbx = din("rpbx", [64, 8, 15, 64]); cmask = din("cmask", [64, 64])
    qkg = din("qkg", [128, 2]); lamv = din("lamv", [128, 4]); subln = din("subln", [128, 2])
    wr = din("wr", [128, 16, 16]); rbias = din("rbias", [128, 64])
    wg = din("wg", [32, D, 1024]); wu = din("wu", [32, D, 1024]); wd = din("wd", [32, 1024, D])
    ropec = din("ropec", [128, 2048]); ropes = din("ropes", [128, 2048])
    perm = din("perm", [128, 128]); ident = din("ident", [128, 128]); trim = din("trim", [128, 2, 4, 128])
    out = nc.dram_tensor("out", [D, NLAT], F32, kind="ExternalOutput").ap()
    xs = dscr("xs", [D, NTOK], F32); fmn = dscr("fmn", [26 * 128, NTOK], BF16); fmr = dscr("fmr", [26 * 128, NLAT], BF16)
    vtok = dscr("vtok", [NTOK, 1280], BF16); oT = dscr("oT", [D, NTOK], BF16); h2T = dscr("h2T", [D, NTOK], BF16)
    gT = dscr("gT", [16, NTOK], F32); fT = dscr("fT", [D, NTOK], F32)
    NSLOT = 33 * 512
    h2tok = dscr("h2tok", [NTOK, D], BF16); h2sorted = dscr("h2sorted", [NSLOT, D], BF16)
    gsorted = dscr("gsorted", [NSLOT, 4], F32); fsorted = dscr("fsorted", [NSLOT, D], BF16)
    ltri = din("ltri", [128, 128]); tgd = din("tgd", [128, 48])
    bh2tok = Buf(); bh2s = Buf(); bgs = Buf(); bfs = Buf()
    bxs = [Buf() for _ in range(9)]
    bfmn = Buf(); bfmr = Buf(); bvtok = Buf(); boT = Buf(); bh2T = Buf(); bgT = Buf(); bfT = Buf(); bout = Buf()

    with contextlib.ExitStack() as top:
        def gsb(n, s, d):
            return top.enter_context(SBT(nc, n, s, d))
        ps = [top.enter_context(nc.psum_tensor("ps%d" % i, [128, 512], F32)) for i in range(8)]
        bps = [Buf() for _ in range(8)]

        class PP:
            i = 0

            @classmethod
            def next(cls, lo=0, hi=8):
                n = hi - lo
                k = lo + (cls.i % n)
                cls.i += 1
                return ps[k], bps[k]

        ones32 = gsb("ones32", [128, 128], F32); onesb = gsb("onesb", [128, 128], BF16)
        perm32 = gsb("perm32", [128, 128], F32); id32 = gsb("id32", [128, 128], F32)
        modT = gsb("modT", [128, 2, 96, 3], F32); lnt = gsb("lnt", [128, 2, 4, 16], F32)
        badat = gsb("badat", [128, 2, 96], F32)
        cint = gsb("cint", [128, 16, 3], F32); cbf = gsb("cbf", [128, 16, 3], BF16)
        qkgt = gsb("qkgt", [128, 2], F32); lamt = gsb("lamt", [128, 8], F32); sublnt = gsb("sublnt", [128, 2], F32)
        wrt = gsb("wrt", [128, 16, 16], F32); rbt = gsb("rbt", [128, 64], F32)
        sinkt = gsb("sinkt", [128, 8], F32)
        ltri32 = gsb("ltri32", [128, 128], F32); idb = gsb("idb", [128, 128], BF16)
        zt = gsb("zt", [128, 2048], BF16); bz = Buf()
        OH = gsb("OH", [128, 36, 16], F32); GS = gsb("GS", [128, 36, 16], F32)
        POSA = gsb("POSA", [128, 36], mybir.dt.int32); POSB = gsb("POSB", [128, 36], mybir.dt.int32)
        GSA = gsb("GSA", [128, 36, 4], F32); GSB = gsb("GSB", [128, 36, 4], F32)
        bOH = Buf(); bGS = Buf(); bPOS = Buf(); bUG = Buf(); bzero = Buf()
        tgt = gsb("tgt", [128, 48], F32)
        IDXG = gsb("IDXG", [128, 33, 32], mybir.dt.int32); IDXD = gsb("IDXD", [128, 33, 8], mybir.dt.int32); bIDX = Buf()
        bconst = Buf(); bmod = Buf(); bc2 = Buf()
        K.memset(ones32[:], 1.0, [bc2]); K.memset(onesb[:], 1.0, [bc2]); K.memset(zt[:], 0.0, [bz])
        for (t_, a_) in ((perm32[:], perm), (id32[:], ident), (lnt[:], lnp), (badat[:], bada), (cint[:], cin), (qkgt[:], qkg),
                         (lamt[:, 0:4], lamv), (sublnt[:], subln), (wrt[:], wr), (rbt[:], rbias), (sinkt[:], sinkb), (ltri32[:], ltri), (tgt[:], tgd)):
            K.dma("sp", t_, a_, [], [bconst], part=True)
        K.cp(idb[:], id32[:], [bconst], [bc2])
        K.act(cbf[:], cint[:], AF.Silu, [bconst], [bc2])
        K.ts(qkgt[:], qkgt[:], math.sqrt(128.0), None, ALU.mult, None, [bconst], [bconst])
        K.ts(sublnt[:], sublnt[:], 16.0 * (1.0 - LAMBDA_INIT1), None, ALU.mult, None, [bconst], [bconst])
        K.act(sinkt[:], sinkt[:], AF.Exp, [bconst], [bconst])
        K.tt(lamt[:, 4:5], lamt[:, 0:1], lamt[:, 1:2], ALU.mult, [bconst], [bconst])
        K.tt(lamt[:, 5:6], lamt[:, 2:3], lamt[:, 3:4], ALU.mult, [bconst], [bconst])
        p_, bp_ = PP.next()
        K.mm(p_[:, 0:2], ones32[:], lamt[:, 4:6], True, True, [bconst, bc2], [bp_])
        K.act(lamt[:, 4:6], p_[:, 0:2], AF.Exp, [bp_], [bconst])
        K.tt(lamt[:, 6:7], lamt[:, 5:6], lamt[:, 4:5], ALU.subtract, [bconst], [bconst])
        K.ts(lamt[:, 6:7], lamt[:, 6:7], -LAMBDA_INIT1, None, ALU.add, None, [bconst], [bconst])

        with contextlib.ExitStack() as ph:
            wsl = [ph.enter_context(SBT(nc, "wa%d" % i, [128, 16, 512], BF16)) for i in range(2)]
            bwsl = [Buf(), Buf()]
            si = 0
            for l in range(2):
                for s in range(24):
                    w_ = si % 2; si += 1
                    K.dma("pool", wsl[w_][:], w_ada[l, :, s * 512:(s + 1) * 512].rearrange("(c p) n -> p c n", p=128), [], [bwsl[w_]])
                    p_, bp_ = PP.next()
                    for oc in range(4):
                        for c in range(16):
                            K.mm(p_[:, oc * 3:(oc + 1) * 3], wsl[w_][:, c, oc * 128:(oc + 1) * 128], cbf[:, c, :], c == 0, c == 15, [bwsl[w_], bc2], [bp_])
                    for j in range(3):
                        K.tt(modT[:, l, 4 * s:4 * s + 4, j], p_[:, j:12:3], badat[:, l, 4 * s:4 * s + 4], ALU.add, [bp_, bconst], [bmod], )
                K.ts(modT[:, l, 16:32, :], modT[:, l, 16:32, :], 1.0, None, ALU.add, None, [bmod], [bmod])
                K.ts(modT[:, l, 64:80, :], modT[:, l, 64:80, :], 1.0, None, ALU.add, None, [bmod], [bmod])
                K.ts(modT[:, l, 32:48, :], modT[:, l, 32:48, :], 1.0 / ALPHA, None, ALU.mult, None, [bmod], [bmod])
                K.ts(modT[:, l, 80:96, :], modT[:, l, 80:96, :], 1.0 / ALPHA, None, ALU.mult, None, [bmod], [bmod])
            S.barrier()
        if dbg:
            dmod = nc.dram_tensor("dmod", [128, 2 * 96 * 3], F32, kind="ExternalOutput").ap()
            K.dma("sp", dmod, modT[:].rearrange("p a b c -> p (a b c)"), [bmod], [Buf()])
            dlam = nc.dram_tensor("dlam", [128, 8], F32, kind="ExternalOutput").ap()
            K.dma("sp", dlam, lamt[:], [bconst], [Buf()])
            S.barrier()
        if dbg:
            I32_ = mybir.dt.int32
            dposa = nc.dram_tensor("dposa", [128, 36], I32_, kind="ExternalOutput").ap()
            dposb = nc.dram_tensor("dposb", [128, 36], I32_, kind="ExternalOutput").ap()
            didxg = nc.dram_tensor("didxg", [128, 33 * 32], I32_, kind="ExternalOutput").ap()
            didxd = nc.dram_tensor("didxd", [128, 33 * 8], I32_, kind="ExternalOutput").ap()
            dgsa = nc.dram_tensor("dgsa", [128, 36 * 4], F32, kind="ExternalOutput").ap()
            doh = nc.dram_tensor("doh", [128, 36 * 16], F32, kind="ExternalOutput").ap()
        C = dict(locals())
        for l in range(2):
            if stop <= 10 * l + 1:
                break
            phase_qkv(C, l)
            if stop <= 10 * l + 2:
                break
            phase_attn(C, l)
            if stop <= 10 * l + 3:
                break
            phase_oproj(C, l)
            phase_route(C, l)
            if stop <= 10 * l + 4:
                break
            phase_moe(C, l)
            if stop <= 10 * l + 5:
                break
            phase_ln2(C, l)
        S.barrier()
        S.emit()
    return nc


_UNIQ = [0]


def SBT(nc, name, shape, dt):
    _UNIQ[0] += 1
    return nc.sbuf_tensor("%s_u%d" % (name, _UNIQ[0]), shape, dt)


class NS:
    def __init__(self, d):
        self.__dict__.update(d)


def phase_qkv(C, l):
    c = NS(C)
    nc, S, K, PP = c.nc, c.S, c.K, c.PP
    src = c.xin if l == 0 else c.xs
    with contextlib.ExitStack() as ph:
        def sb(n, s, d):
            return ph.enter_context(SBT(nc, n, s, d))

        def pair(n, s, d, depth=2):
            return [sb("%s%d" % (n, i), s, d) for i in range(depth)], [Buf() for _ in range(depth)]
        QD = 4
        x32, bx32 = pair("x32_", [128, 16, 512], F32, 1)
        x32 = x32 * 2; bx32 = bx32 * 2
        hb, bhb = pair("hb_", [128, 16, 512], BF16)
        ws, bws = pair("ws_", [128, 16, 512], BF16)
        rc = sb("rc", [128, 2048], F32); rs_ = sb("rs", [128, 2048], F32); brc = Buf(); brs = Buf()
        q32, bq32 = pair("q32_", [128, 512], F32, QD)
        sq, bsq = pair("sq_", [128, 512], F32, QD)
        rr, brr = pair("rr_", [128, 512], F32, QD)
        t1, bt1 = pair("t1_", [128, 512], F32, QD)
        t2, bt2 = pair("t2_", [128, 512], F32, QD)
        stn, bstn = pair("stn_", [128, 512], BF16, QD)
        str_, bstr = pair("str_", [128, 512], BF16, QD)
        vst, bvst = pair("vst_", [128, 512], BF16)
        K.dma("sp", rc[:], c.ropec, [], [brc]); K.dma("sp", rs_[:], c.ropes, [], [brs])
        si = 0; qi = 0; vi = 0
        for tt in range(9):
            lat = tt < 8
            j = tt // 4 if lat else 2
            tok0 = (tt % 4) * 512
            xb = tt % 2
            cols = slice(tt * 512, (tt + 1) * 512)
            K.dma("sp", x32[xb][:], src[:, cols].rearrange("(c p) t -> p c t", p=128), [c.bxs[tt]], [bx32[xb]])
            for k in range(16):
                K.ts(hb[xb][:, k, :], x32[xb][:, k, :], c.modT[:, l, 16 + k, j:j + 1], c.modT[:, l, k, j:j + 1],
                     ALU.mult, ALU.add, [bx32[xb], c.bmod], [bhb[xb]], part=True)
            slabs = list(range(9)) if (lat or l == 0) else [2, 5, 6, 7, 8]
            for s in slabs:
                w_ = si % 2; si += 1
                K.dma("pool", ws[w_][:], c.w_in[l][:, s * 512:(s + 1) * 512].rearrange("(c p) n -> p c n", p=128), [], [bws[w_]])
                if s in (2, 7, 8):
                    c0, ncol = (256, 256) if s == 2 else (0, 512)
                    vcol = 0 if s == 2 else 256 + (s - 7) * 512
                    for tb in range(4):
                        p_, bp_ = PP.next()
                        for k in range(16):
                            K.mm(p_[:, 0:ncol], hb[xb][:, k, tb * 128:(tb + 1) * 128], ws[w_][:, k, c0:c0 + ncol], k == 0, k == 15,
                                 [bhb[xb], bws[w_]], [bp_])
                        v = vi % 2; vi += 1
                        K.act(vst[v][:, 0:ncol], p_[:, 0:ncol], AF.Copy, [bp_], [bvst[v]])
                        r0 = tt * 512 + tb * 128
                        K.dma("sp", c.vtok[r0:r0 + 128, vcol:vcol + ncol], vst[v][:, 0:ncol], [bvst[v]], [c.bvtok], part=True)
                fch = [0, 1, 2, 3] if s not in (2, 7, 8) else ([0, 1] if s == 2 else [])
                items = []
                for ci in fch:
                    co = s * 4 + ci
                    if l == 1 and (not lat) and (co < 8 or 12 <= co < 20):
                        continue
                    fi = co if co < 10 else co - 2
                    norm = (l == 1 and co < 10)
                    rope = lat and (co < 10 or l == 1)
                    nopos = (not lat) or co < 8 or (12 <= co < 20) or (l == 0 and co >= 20)
                    p_, bp_ = PP.next()
                    for k in range(16):
                        K.mm(p_[:], ws[w_][:, k, ci * 128:(ci + 1) * 128], hb[xb][:, k, :], k == 0, k == 15, [bhb[xb], bws[w_]], [bp_])
                    q = qi % QD; qi += 1
                    items.append(dict(co=co, fi=fi, norm=norm, rope=rope, nopos=nopos, p=p_, bp=bp_, q=q))
                for it in items:
                    q = it["q"]
                    K.act(q32[q][:], it["p"][:], AF.Copy, [it["bp"]], [bq32[q]])
                nl = [it for it in items if it["norm"]]
                for it in nl:
                    q = it["q"]
                    K.act(sq[q][:], q32[q][:], AF.Square, [bq32[q]], [bsq[q]])
                for it in nl:
                    q = it["q"]
                    it["p2"], it["bp2"] = PP.next()
                    K.mm(it["p2"][:], c.ones32[:], sq[q][:], True, True, [bsq[q], c.bc2], [it["bp2"]])
                for it in nl:
                    q = it["q"]
                    K.ts(rr[q][:], it["p2"][:], 128.0 * 1e-6, None, ALU.add, None, [it["bp2"]], [brr[q]])
                for it in nl:
                    q = it["q"]
                    K.act(rr[q][:], rr[q][:], AF.Sqrt, [brr[q]], [brr[q]])
                for it in nl:
                    q = it["q"]
                    K.recip(rr[q][:], rr[q][:], [brr[q]], [brr[q]])
                for it in nl:
                    q = it["q"]
                    gcol = c.qkgt[:, 0:1] if it["co"] < 8 else c.qkgt[:, 1:2]
                    K.stt(q32[q][:], q32[q][:], gcol, rr[q][:], ALU.mult, ALU.mult, [bq32[q], brr[q], c.bconst], [bq32[q]])
                for it in items:
                    if it["nopos"]:
                        q = it["q"]
                        K.act(stn[q][:], q32[q][:], AF.Copy, [bq32[q]], [bstn[q]])
                        K.dma("sp", c.fmn[it["fi"] * 128:(it["fi"] + 1) * 128, cols], stn[q][:], [bstn[q]], [c.bfmn], part=True)
                rl = [it for it in items if it["rope"]]
                for it in rl:
                    q = it["q"]
                    it["p3"], it["bp3"] = PP.next()
                    K.mm(it["p3"][:], c.perm32[:], q32[q][:], True, True, [bq32[q], c.bconst], [it["bp3"]])
                for it in rl:
                    q = it["q"]
                    K.tt(t1[q][:], q32[q][:], rc[:, tok0:tok0 + 512], ALU.mult, [bq32[q], brc], [bt1[q]])
                for it in rl:
                    q = it["q"]
                    K.tt(t2[q][:], it["p3"][:], rs_[:, tok0:tok0 + 512], ALU.mult, [it["bp3"], brs], [bt2[q]])
                for it in rl:
                    q = it["q"]
                    K.tt(str_[q][:], t1[q][:], t2[q][:], ALU.add, [bt1[q], bt2[q]], [bstr[q]])
                    K.dma("sp", c.fmr[it["fi"] * 128:(it["fi"] + 1) * 128, cols], str_[q][:], [bstr[q]], [c.bfmr], part=True)
        S.barrier()


class AttnCtx:
    def __init__(self, c, ph, nS=3):
        self.c = c
        nc = c.nc
        self.nS = nS
        self.NP = 6
        self.P = [ph.enter_context(SBT(nc, "Pb%d" % i, [128, 512], BF16)) for i in range(6)]
        self.bP = [Buf() for _ in range(6)]
        self.pi = 0
        self.si = 0
        self.set = 0

    def block(self, N, kbs, nv, shape3=None):
        c = self.c
        K = c.K
        nS = self.nS
        if nv == 1:
            base = nS + 2 * (self.set % ((8 - nS) // 2))
            o = [(c.ps[base], c.bps[base])]
            sm = (c.ps[base + 1], c.bps[base + 1])
        else:
            base = nS
            o = [(c.ps[base + i], c.bps[base + i]) for i in range(nv)]
            sm = (c.ps[base + nv], c.bps[base + nv])
        aset = self.set % 2
        self.set += 1
        n = len(kbs)
        skew = nS - 1

        def view(ap):
            return ap

        def issue_s(i):
            kk, qq, vv, mask, reads, kp = kbs[i]
            sp_, bsp_ = c.ps[self.si % nS], c.bps[self.si % nS]
            self.si += 1
            K.mm(sp_[0:kp, 0:N], kk, qq, True, True, reads, [bsp_])
            pb = self.pi % self.NP
            self.pi += 1
            K.act(self.P[pb][0:kp, 0:N], sp_[0:kp, 0:N], AF.Exp, [bsp_], [self.bP[pb]], scale=SCALE)
            if mask is not None:
                K.tt(self.P[pb][0:kp, 0:N], self.P[pb][0:kp, 0:N], mask, ALU.mult, [self.bP[pb], c.bconst], [self.bP[pb]])
            return pb
        pend = [issue_s(i) for i in range(min(skew, n))]
        for i in range(n):
            if i + skew < n:
                pend.append(issue_s(i + skew))
            pb = pend[i]
            kk, qq, vv, mask, reads, kp = kbs[i]
            for vi in range(nv):
                K.mm(o[vi][0][:, 0:N], vv[vi], self.P[pb][0:kp, 0:N], i == 0, i == n - 1, [self.bP[pb]] + reads, [o[vi][1]])
            K.mm(sm[0][:, 0:N], c.onesb[0:kp, :], self.P[pb][0:kp, 0:N], i == 0, i == n - 1, [self.bP[pb], c.bc2], [sm[1]])
        return o, sm


def load_fm(c, q, dst, src_rows, src, col0, ncol, rbuf, wbuf, part=False):
    r0, r1 = src_rows
    c.K.dma(q, dst, src[r0 * 128:r1 * 128, col0:col0 + ncol].rearrange("(h p) t -> p h t", p=128), [rbuf], [wbuf], part=part)


def gqa_attn(c, A, ph, l, b, qrot, qnop, krot, kx, vA, bl, stg, bstg, rden, brden, sinkx, oc0):
    K = c.K
    n_ = [0]
    for g in range(2):
        qlist = [("lat", qb) for qb in range(16)] + ([("ctx", qb) for qb in range(2)] if l == 0 else [])
        for kind, qb in qlist:
            kbs = []
            if kind == "lat":
                lk = [kb for kb in (qb - 1, qb, qb + 1) if 0 <= kb < 16] if l == 0 else list(range(16))
                for kb in lk:
                    mask = None
                    if l == 0 and kb == qb - 1:
                        mask = c.trimb[:, 0, :, :].rearrange("p h q -> p (h q)")
                    if l == 0 and kb == qb + 1:
                        mask = c.trimb[:, 1, :, :].rearrange("p h q -> p (h q)")
                    kbs.append((krot[:, g, kb * 128:(kb + 1) * 128], qrot[:, 4 * g:4 * g + 4, qb * 128:(qb + 1) * 128],
                                [vA[:, kb, g * 128:(g + 1) * 128]], mask, [bl], 128))
                qn = qnop[:, 4 * g:4 * g + 4, qb * 128:(qb + 1) * 128]
                tcol = b * 2048 + qb * 128
            else:
                qn = qnop[:, 4 * g:4 * g + 4, 2048 + qb * 128:2048 + (qb + 1) * 128]
                tcol = 4096 + b * 256 + qb * 128
            for cb in range(2):
                kbs.append((kx[:, g, cb * 128:(cb + 1) * 128], qn, [vA[:, 16 + cb, g * 128:(g + 1) * 128]], None, [bl], 128))
            o, sm = A.block(512, kbs, 1)
            i = n_[0] % 2
            n_[0] += 1
            if l == 0:
                K.tt(rden[i][:], sm[0][:], sinkx[:, g, :], ALU.add, [sm[1], c.bconst], [brden[i]])
                K.recip(rden[i][:], rden[i][:], [brden[i]], [brden[i]])
            else:
                K.recip(rden[i][:], sm[0][:], [sm[1]], [brden[i]])
            K.tt(stg[i][:], o[0][0][:], rden[i][:], ALU.mult, [o[0][1], brden[i]], [bstg[i]])
            K.dma("sp", c.oT[(oc0 + 4 * g) * 128:(oc0 + 4 * g + 4) * 128, tcol:tcol + 128].rearrange("(h p) t -> p h t", p=128),
                  stg[i][:].rearrange("p (h q) -> p h q", h=4), [bstg[i]], [c.boT], part=True)


def phase_attn(C, l):
    c = NS(C)
    nc, S, K, PP = c.nc, c.S, c.K, c.PP
    for b in range(2):
        with contextlib.ExitStack() as ph:
            def sb(n, s, d):
                return ph.enter_context(SBT(nc, n, s, d))
            A = AttnCtx(c, ph)
            qrot = sb("qrot", [128, 8, 2048], BF16); qnop = sb("qnop", [128, 8, 2304], BF16)
            krot = sb("krot", [128, 2, 2048], BF16); kx = sb("kx", [128, 2, 256], BF16)
            vA = sb("vA", [128, 18, 256], BF16)
            trimb = sb("trimb", [128, 2, 4, 128], BF16); trim32 = sb("trim32", [128, 2, 4, 128], F32)
            sinkx = sb("sinkx", [128, 2, 512], F32)
            stg = [sb("stg%d" % i, [128, 512], BF16) for i in range(2)]; bstg = [Buf(), Buf()]
            rden = [sb("rden%d" % i, [128, 512], F32) for i in range(2)]; brden = [Buf(), Buf()]
            bl = Buf(); bt_ = Buf()
            c.trimb = trimb
            K.dma("sp", trim32[:], c.trim, [], [bt_])
            K.cp(trimb[:], trim32[:], [bt_], [c.bconst])
            for h in range(8):
                K.ts(sinkx[:, h // 4, (h % 4) * 128:(h % 4 + 1) * 128], c.ones32[:], c.sinkt[:, h:h + 1], None, ALU.mult, None,
                     [c.bconst, c.bc2], [c.bconst])
            load_fm(c, "sp", qrot[:], (0, 8), c.fmr, b * 2048, 2048, c.bfmr, bl, part=True)
            load_fm(c, "sp", qnop[:, :, 0:2048], (0, 8), c.fmn, b * 2048, 2048, c.bfmn, bl, part=True)
            if l == 0:
                load_fm(c, "sp", qnop[:, :, 2048:2304], (0, 8), c.fmn, 4096 + b * 256, 256, c.bfmn, bl, part=True)
            load_fm(c, "sp", krot[:], (8, 10), c.fmr, b * 2048, 2048, c.bfmr, bl, part=True)
            load_fm(c, "sp", kx[:], (8, 10), c.fmn, 4096 + b * 256, 256, c.bfmn, bl, part=True)
            K.dma("sp", vA[:, 0:16, :], c.vtok[b * 2048:(b + 1) * 2048, 0:256].rearrange("(k p) n -> p k n", p=128), [c.bvtok], [bl], part=True)
            K.dma("sp", vA[:, 16:18, :], c.vtok[4096 + b * 256:4096 + (b + 1) * 256, 0:256].rearrange("(k p) n -> p k n", p=128), [c.bvtok], [bl], part=True)
            if l == 0 and b == 0:
                for i in range(33 * 4):
                    K.dma("sp", c.h2sorted[i * 128:(i + 1) * 128, :], c.zt[:], [c.bz], [c.bzero], part=True)
            gqa_attn(c, A, ph, l, b, qrot, qnop, krot, kx, vA, bl, stg, bstg, rden, brden, sinkx, 0)
            S.barrier()
        if l == 0:
            attn_B(c, b)
        else:
            attn_D(c, b)


def attn_B(c, b):
    nc, S, K = c.nc, c.S, c.K
    with contextlib.ExitStack() as ph:
        def sb(n, s, d):
            return ph.enter_context(SBT(nc, n, s, d))
        A = AttnCtx(c, ph, 2)
        qB = sb("qB", [128, 8, 2304], BF16); kB = sb("kB", [128, 8, 2304], BF16)
        vB64 = sb("vB64", [64, 32, 1024], BF16); vBx = sb("vBx", [128, 2, 1024], BF16)
        Tt = sb("Tt", [64, 8, 15, 64], BF16)
        rp32 = [sb("rp32_%d" % i, [64, 15, 64], F32) for i in range(2)]; brp = [Buf(), Buf()]
        cm = sb("cm", [64, 64], F32); bcm = Buf(); bT = Buf(); bl = Buf()
        Pl = [sb("Pl%d" % i, [64, 512], BF16) for i in range(2)]; bPl = [Buf(), Buf()]
        Pc = [sb("Pc%d" % i, [128, 128], BF16) for i in range(2)]; bPc = [Buf(), Buf()]
        stg = [sb("stgB%d" % i, [128, 512], BF16) for i in range(2)]; bstg = [Buf(), Buf()]
        rden = [sb("rdenB%d" % i, [128, 512], F32) for i in range(2)]; brden = [Buf(), Buf()]
        K.dma("sp", cm[:], c.cmask, [], [bcm])
        for h in range(8):
            i = h % 2
            K.dma("sp", rp32[i][:], c.rpbx[:, h, :, :], [], [brp[i]])
            K.act(rp32[i][:], rp32[i][:], AF.Exp, [brp[i]], [brp[i]])
            K.tt(Tt[:, h, :, :], rp32[i][:], cm[:].unsqueeze(1).to_broadcast([64, 15, 64]), ALU.mult, [brp[i], bcm], [bT], part=True)
        load_fm(c, "sp", qB[:, :, 0:2048], (10, 18), c.fmn, b * 2048, 2048, c.bfmn, bl, part=True)
        load_fm(c, "sp", qB[:, :, 2048:2304], (10, 18), c.fmn, 4096 + b * 256, 256, c.bfmn, bl, part=True)
        load_fm(c, "sp", kB[:, :, 0:2048], (18, 26), c.fmn, b * 2048, 2048, c.bfmn, bl, part=True)
        load_fm(c, "sp", kB[:, :, 2048:2304], (18, 26), c.fmn, 4096 + b * 256, 256, c.bfmn, bl, part=True)
        for hh in range(2):
            K.dma("sp", vB64[:, hh * 16:(hh + 1) * 16, :],
                  c.vtok[b * 2048 + hh * 1024:b * 2048 + (hh + 1) * 1024, 256:1280].rearrange("(r p) n -> p r n", p=64), [c.bvtok], [bl], part=True)
        K.dma("sp", vBx[:], c.vtok[4096 + b * 256:4096 + (b + 1) * 256, 256:1280].rearrange("(k p) n -> p k n", p=128), [c.bvtok], [bl], part=True)
        items = [(h, rg, r) for h in range(8) for rg in range(4) for r in range(rg * 8, rg * 8 + 8)]

        def stage_s(n_):
            h, rg, r = items[n_]
            r0 = min(max(r - 4, 0), 24)
            d0 = r0 - r + 7
            i = n_ % 2
            sl, bsl = c.ps[i], c.bps[i]
            sc_, bsc = c.ps[2 + i], c.bps[2 + i]
            qq = qB[:, h, r * 64:(r + 1) * 64]
            for kr in range(8):
                K.mm(sl[0:64, kr * 64:(kr + 1) * 64], kB[:, h, (r0 + kr) * 64:(r0 + kr + 1) * 64], qq, True, True, [bl], [bsl], part=True)
            for cb in range(2):
                K.mm(sc_[:, cb * 64:(cb + 1) * 64], kB[:, h, 2048 + cb * 128:2048 + (cb + 1) * 128], qq, True, True, [bl], [bsc], part=True)
            K.act(Pl[i][:], sl[0:64, :], AF.Exp, [bsl], [bPl[i]], scale=SCALE)
            K.act(Pc[i][:], sc_[:, 0:128], AF.Exp, [bsc], [bPc[i]], scale=SCALE)
            K.tt(Pl[i][:].rearrange("p (a q) -> p a q", a=8), Pl[i][:].rearrange("p (a q) -> p a q", a=8), Tt[:, h, d0:d0 + 8, :], ALU.mult,
                 [bPl[i], bT], [bPl[i]])

        def stage_pv(n_):
            h, rg, r = items[n_]
            r0 = min(max(r - 4, 0), 24)
            i = n_ % 2
            st = (h * 4 + rg) % 2
            o_ps, bo = c.ps[4 + 2 * st], c.bps[4 + 2 * st]
            s_ps, bs = c.ps[5 + 2 * st], c.bps[5 + 2 * st]
            oc = slice((r % 8) * 64, (r % 8 + 1) * 64)
            for kr in range(8):
                K.mm(o_ps[:, oc], vB64[:, r0 + kr, h * 128:(h + 1) * 128], Pl[i][:, kr * 64:(kr + 1) * 64], kr == 0, False, [bPl[i], bl], [bo], part=True)
            for cb in range(2):
                K.mm(o_ps[:, oc], vBx[:, cb, h * 128:(h + 1) * 128], Pc[i][:, cb * 64:(cb + 1) * 64], False, cb == 1, [bPc[i], bl], [bo], part=True)
            for kr in range(8):
                K.mm(s_ps[:, oc], c.onesb[0:64, :], Pl[i][:, kr * 64:(kr + 1) * 64], kr == 0, False, [bPl[i], c.bc2], [bs], part=True)
            for cb in range(2):
                K.mm(s_ps[:, oc], c.onesb[:, :], Pc[i][:, cb * 64:(cb + 1) * 64], False, cb == 1, [bPc[i], c.bc2], [bs], part=True)
            if r % 8 == 7:
                K.recip(rden[st][:], s_ps[:], [bs], [brden[st]])
                K.tt(stg[st][:], o_ps[:], rden[st][:], ALU.mult, [bo, brden[st]], [bstg[st]])
                tcol = b * 2048 + rg * 512
                K.dma("sp", c.oT[(8 + h) * 128:(9 + h) * 128, tcol:tcol + 512], stg[st][:], [bstg[st]], [c.boT], part=True)
        stage_s(0)
        for n_ in range(len(items)):
            if n_ + 1 < len(items):
                stage_s(n_ + 1)
            stage_pv(n_)
        for h in range(8):
            kbs = []
            for cb in range(2):
                kbs.append((kB[:, h, 2048 + cb * 128:2048 + (cb + 1) * 128], qB[:, h, 2048:2304], [vBx[:, cb, h * 128:(h + 1) * 128]], None, [bl], 128))
            o, sm = A.block(256, kbs, 1)
            st = h % 2
            K.recip(rden[st][:, 0:256], sm[0][:, 0:256], [sm[1]], [brden[st]])
            K.tt(stg[st][:, 0:256], o[0][0][:, 0:256], rden[st][:, 0:256], ALU.mult, [o[0][1], brden[st]], [bstg[st]])
            tcol = 4096 + b * 256
            K.dma("sp", c.oT[(8 + h) * 128:(9 + h) * 128, tcol:tcol + 256], stg[st][:, 0:256], [bstg[st]], [c.boT], part=True)
        S.barrier()


def attn_D(c, b):
    nc, S, K = c.nc, c.S, c.K
    with contextlib.ExitStack() as ph:
        def sb(n, s, d):
            return ph.enter_context(SBT(nc, n, s, d))
        A = AttnCtx(c, ph, 4)
        dq = sb("dq", [128, 8, 2048], BF16); dqn = sb("dqn", [128, 8, 2048], BF16)
        dk = sb("dk", [128, 8, 2048], BF16); dkx = sb("dkx", [128, 8, 256], BF16)
        vD = sb("vD", [128, 18, 1024], BF16)
        o0 = sb("o0", [128, 2, 512], F32); od = sb("od", [128, 2, 512], F32); sqd = sb("sqd", [128, 2, 512], F32)
        r0_ = sb("r0_", [128, 512], F32); r1_ = sb("r1_", [128, 512], F32); rs2 = sb("rs2", [128, 512], F32)
        stg = [sb("stgD%d" % i, [128, 512], BF16) for i in range(2)]; bstg = [Buf(), Buf()]
        bl = Buf(); bo0 = Buf(); bod = Buf(); bsq = Buf(); br0 = Buf(); br1 = Buf(); brs2 = Buf()
        load_fm(c, "sp", dq[:], (10, 18), c.fmr, b * 2048, 2048, c.bfmr, bl, part=True)
        load_fm(c, "sp", dqn[:], (10, 18), c.fmn, b * 2048, 2048, c.bfmn, bl, part=True)
        load_fm(c, "sp", dk[:], (18, 26), c.fmr, b * 2048, 2048, c.bfmr, bl, part=True)
        load_fm(c, "sp", dkx[:], (18, 26), c.fmn, 4096 + b * 256, 256, c.bfmn, bl, part=True)
        for hh in range(2):
            K.dma("sp", vD[:, hh * 8:(hh + 1) * 8, :],
                  c.vtok[b * 2048 + hh * 1024:b * 2048 + (hh + 1) * 1024, 256:1280].rearrange("(k p) n -> p k n", p=128), [c.bvtok], [bl], part=True)
        K.dma("sp", vD[:, 16:18, :], c.vtok[4096 + b * 256:4096 + (b + 1) * 256, 256:1280].rearrange("(k p) n -> p k n", p=128), [c.bvtok], [bl], part=True)
        n_ = 0
        for hd in range(4):
            for qt in range(4):
                qs = slice(qt * 512, (qt + 1) * 512)
                for t in range(2):
                    cc = 2 * hd + t
                    kbs = []
                    for kb in range(16):
                        kbs.append((dk[:, cc, kb * 128:(kb + 1) * 128], dq[:, cc, qs],
                                    [vD[:, kb, hd * 256:hd * 256 + 128], vD[:, kb, hd * 256 + 128:hd * 256 + 256]], None, [bl], 128))
                    for cb in range(2):
                        kbs.append((dkx[:, cc, cb * 128:(cb + 1) * 128], dqn[:, cc, qs],
                                    [vD[:, 16 + cb, hd * 256:hd * 256 + 128], vD[:, 16 + cb, hd * 256 + 128:hd * 256 + 256]], None, [bl], 128))
                    o, sm = A.block(512, kbs, 2)
                    if t == 0:
                        K.recip(r0_[:], sm[0][:], [sm[1]], [br0])
                        for vi in range(2):
                            K.tt(o0[:, vi, :], o[vi][0][:], r0_[:], ALU.mult, [o[vi][1], br0], [bo0], part=(vi == 1))
                    else:
                        K.recip(r1_[:], sm[0][:], [sm[1]], [br1])
                        K.ts(r1_[:], r1_[:], c.lamt[:, 6:7], None, ALU.mult, None, [br1, c.bconst], [br1])
                        for vi in range(2):
                            K.tt(od[:, vi, :], o[vi][0][:], r1_[:], ALU.mult, [o[vi][1], br1], [bod], part=(vi == 1))
                        K.tt(od[:], od[:], o0[:], ALU.add, [bod, bo0], [bod])
                K.act(sqd[:], od[:], AF.Square, [bod], [bsq])
                p_, bp_ = c.ps[7], c.bps[7]
                K.mm(p_[:], c.ones32[:], sqd[:, 0, :], True, False, [bsq, c.bc2], [bp_])
                K.mm(p_[:], c.ones32[:], sqd[:, 1, :], False, True, [bsq, c.bc2], [bp_])
                K.rsqrt(rs2[:], p_[:], 256.0 * 1e-6, [bp_], [brs2])
                for vi in range(2):
                    i = n_ % 2
                    n_ += 1
                    K.stt(stg[i][:], od[:, vi, :], c.sublnt[:, vi:vi + 1], rs2[:], ALU.mult, ALU.mult, [bod, brs2, c.bconst], [bstg[i]])
                    tcol = b * 2048 + qt * 512
                    K.dma("sp", c.oT[(8 + 2 * hd + vi) * 128:(9 + 2 * hd + vi) * 128, tcol:tcol + 512], stg[i][:], [bstg[i]], [c.boT], part=True)
        S.barrier()


def ln_tile(c, K, x32, bxk, lcol, gcol_fn, eps, zsq, bzsq, mean, msq, var, rstd, bst, post):
    p_s, bp_s = c.ps[6], c.bps[6]
    p_q, bp_q = c.ps[7], c.bps[7]
    for k in range(16):
        i = k % 2
        K.act(zsq[i][:], x32[:, k, :], AF.Square, [bxk[k]], [bzsq[i]])
        K.mm(p_s[:], c.ones32[:], x32[:, k, :], k == 0, k == 15, [bxk[k], c.bc2], [bp_s])
        K.mm(p_q[:], c.ones32[:], zsq[i][:], k == 0, k == 15, [bzsq[i], c.bc2], [bp_q])
    K.ts(mean[:], p_s[:], 1.0 / D, None, ALU.mult, None, [bp_s], [bst])
    K.tt(msq[:], mean[:], mean[:], ALU.mult, [bst], [bst])
    K.stt(var[:], p_q[:], 1.0 / D, msq[:], ALU.mult, ALU.subtract, [bp_q, bst], [bst])
    K.rsqrt(rstd[:], var[:], eps, [bst], [bst])
    for k in range(16):
        K.tt(x32[:, k, :], x32[:, k, :], mean[:], ALU.subtract, [bxk[k], bst], [bxk[k]])
        K.tt(x32[:, k, :], x32[:, k, :], rstd[:], ALU.mult, [bxk[k], bst], [bxk[k]])
        post(k)


def phase_oproj(C, l):
    c = NS(C)
    nc, S, K, PP = c.nc, c.S, c.K, c.PP
    src = c.xin if l == 0 else c.xs
    ntile = 9 if l == 0 else 8
    with contextlib.ExitStack() as ph:
        def sb(n, s, d):
            return ph.enter_context(SBT(nc, n, s, d))
        wo = sb("wo", [128, 16, 2048], BF16); bwo = Buf()
        for i in range(4):
            K.dma("pool", wo[:, 4 * i:4 * i + 4, :], c.w_out[l][512 * i:512 * (i + 1), :].rearrange("(c p) n -> p c n", p=128), [], [bwo], part=True)
        x32 = sb("x32o", [128, 16, 512], F32); bxk = [Buf() for _ in range(16)]
        h232 = sb("h232", [128, 16, 512], F32); bh2 = Buf()
        ot = [sb("ot%d" % i, [128, 16, 512], BF16) for i in range(2)]; bot = [Buf(), Buf()]
        zsq = [sb("zsq%d" % i, [128, 512], F32) for i in range(2)]; bzsq = [Buf(), Buf()]
        mean = sb("mean", [128, 512], F32); msq = sb("msq", [128, 512], F32); var = sb("var", [128, 512], F32); rstd = sb("rstd", [128, 512], F32)
        bst = Buf()
        sc = sb("rsc", [128, 64], F32); bi = sb("rbi", [128, 64], F32); tmp = sb("rtmp", [128, 64], F32); mk = sb("rmk", [128, 64], F32)
        m1 = sb("rm1", [128, 16], F32); m2 = sb("rm2", [128, 16], F32); gs = sb("rgs", [128, 16], F32); pen = sb("rpen", [128, 16], F32)
        gm = sb("rgm", [128, 4], F32); t1 = sb("rt1", [128, 4], F32); t2 = sb("rt2", [128, 4], F32); wsum = sb("rws", [128, 4], F32)
        gates = sb("gates", [128, 64], F32)
        h2tm = [sb("h2tm%d" % i, [128, 2048], BF16) for i in range(2)]; bh2tm = [Buf(), Buf()]
        br = Buf(); bgt = Buf()
        for tt in range(ntile):
            lat = tt < 8
            j = tt // 4 if lat else 2
            cols = slice(tt * 512, (tt + 1) * 512)
            for hk in range(2):
                K.dma("sp", x32[:, hk * 8:(hk + 1) * 8, :], src[hk * 1024:(hk + 1) * 1024, cols].rearrange("(c p) t -> p c t", p=128), [c.bxs[tt]], bxk[hk * 8:(hk + 1) * 8])
            o_ = tt % 2
            K.dma("sp", ot[o_][:], c.oT[:, cols].rearrange("(c p) t -> p c t", p=128), [c.boT], [bot[o_]])
            for co in range(16):
                p_, bp_ = PP.next(0, 6)
                for k in range(16):
                    K.mm(p_[:], wo[:, k, co * 128:(co + 1) * 128], ot[o_][:, k, :], k == 0, k == 15, [bwo, bot[o_]], [bp_])
                K.stt(x32[:, co, :], p_[:], c.modT[:, l, 32 + co, j:j + 1], x32[:, co, :], ALU.mult, ALU.add, [bp_, c.bmod, bxk[co]], [bxk[co]])

            def post(k):
                K.act(x32[:, k, :], x32[:, k, :], AF.Identity, [bxk[k], c.bconst], [bxk[k]], bias=c.lnt[:, l, 1, k:k + 1], scale=c.lnt[:, l, 0, k:k + 1])
                K.act(h232[:, k, :], x32[:, k, :], AF.Identity, [bxk[k], c.bmod], [bh2], bias=c.modT[:, l, 48 + k, j:j + 1], scale=c.modT[:, l, 64 + k, j:j + 1], part=True)
                K.cp(ot[o_][:, k, :], h232[:, k, :], [bh2], [bot[o_]], part=True)
            ln_tile(c, K, x32, bxk, None, None, LN_EPS, zsq, bzsq, mean, msq, var, rstd, bst, post)
            K.dma("sp", c.xs[:, cols].rearrange("(c p) t -> p c t", p=128), x32[:], bxk, [c.bxs[tt]])
            for tb in range(4):
                m_ = tb % 2
                for hf in range(2):
                    p_, bp_ = PP.next(0, 6)
                    pv = p_[:].bitcast(BF16)
                    for kk in range(8):
                        k = hf * 8 + kk
                        K.tr(pv[:, kk * 128:(kk + 1) * 128], ot[o_][:, k, tb * 128:(tb + 1) * 128], c.idb[:], [bot[o_], c.bc2], [bp_], part=True)
                    K.cp(h2tm[m_][:, hf * 1024:(hf + 1) * 1024], pv[:, 0:1024], [bp_], [bh2tm[m_]], part=(hf == 1))
                r0 = tt * 512 + tb * 128
                K.dma("sp", c.h2tok[r0:r0 + 128, :], h2tm[m_][:], [bh2tm[m_]], [c.bh2tok], part=True)
            for tb in range(4):
                p_, bp_ = PP.next(0, 6)
                for k in range(16):
                    K.mm(p_[:, 0:16], h232[:, k, tb * 128:(tb + 1) * 128], c.wrt[:, k, :], k == 0, k == 15, [bh2, c.bconst], [bp_])
                K.act(sc[:, tb * 16:(tb + 1) * 16], p_[:, 0:16], AF.Sigmoid, [bp_], [br], part=True)
            v3 = lambda t: t[:].rearrange("p (a e) -> p a e", e=4)
            v16 = lambda t: t[:].rearrange("p (a e) -> p a e", e=16)
            R = [br, c.bconst]
            K.tt(bi[:], sc[:], c.rbt[:], ALU.add, R, [br])
            K.red(m1[:], v3(bi), ALU.max, R, [br])
            K.tt(v3(tmp), v3(bi), m1[:].unsqueeze(2).to_broadcast([128, 16, 4]), ALU.is_equal, R, [br])
            K.stt(tmp[:], tmp[:], -BIG, bi[:], ALU.mult, ALU.add, R, [br])
            K.red(m2[:], v3(tmp), ALU.max, R, [br])
            K.tt(gs[:], m1[:], m2[:], ALU.add, R, [br])
            K.red(gm[:], gs[:].rearrange("p (a g) -> p a g", g=4), ALU.max, R, [br])
            K.tt(pen[:].rearrange("p (a g) -> p a g", g=4), gs[:].rearrange("p (a g) -> p a g", g=4),
                 gm[:].unsqueeze(2).to_broadcast([128, 4, 4]), ALU.is_ge, R, [br])
            K.ts(pen[:], pen[:], BIG, -BIG, ALU.mult, ALU.add, R, [br])
            K.tt(v3(mk), v3(bi), pen[:].unsqueeze(2).to_broadcast([128, 16, 4]), ALU.add, R, [br])
            K.red(t1[:], v16(mk), ALU.max, R, [br])
            K.tt(v16(tmp), v16(mk), t1[:].unsqueeze(2).to_broadcast([128, 4, 16]), ALU.is_equal, R, [br])
            K.stt(tmp[:], tmp[:], -BIG, mk[:], ALU.mult, ALU.add, R, [br])
            K.red(t2[:], v16(tmp), ALU.max, R, [br])
            K.tt(v16(tmp), v16(mk), t2[:].unsqueeze(2).to_broadcast([128, 4, 16]), ALU.is_ge, R, [br])
            K.cp(c.OH[:, tt * 4:(tt + 1) * 4, :], v16(tmp), R, [c.bOH], part=True)
            K.tt(tmp[:], tmp[:], sc[:], ALU.mult, R, [br])
            K.red(wsum[:], v16(tmp), ALU.add, R, [br])
            K.recip(wsum[:], wsum[:], R, [br])
            K.tt(v16(gates), v16(tmp), wsum[:].unsqueeze(2).to_broadcast([128, 4, 16]), ALU.mult, R, [br])
            K.cp(c.GS[:, tt * 4:(tt + 1) * 4, :], v16(gates), R, [c.bGS], part=True)
        S.barrier()


def phase_route(C, l):
    c = NS(C)
    nc, S, K, PP = c.nc, c.S, c.K, c.PP
    nblk = 36 if l == 0 else 32
    NU = 33 if l == 0 else 31
    with contextlib.ExitStack() as ph:
        def sb(n, s, d):
            return ph.enter_context(SBT(nc, n, s, d))
        R = sb("R", [128, 36, 16], F32); Bc = sb("Bc", [128, 36, 16], F32); run = sb("run", [128, 37, 16], F32)
        val = sb("val", [128, 36, 16], F32); valm = sb("valm", [128, 36, 16], F32); isA = sb("isA", [128, 36, 16], F32)
        pA = sb("pA", [128, 36], F32); pB = sb("pB", [128, 36], F32); gA = sb("gA", [128, 36], F32); gT_ = sb("gTt", [128, 36], F32)
        nun = sb("nun", [128, 16], F32); off = sb("off", [128, 16], F32); cmp3 = sb("cmp3", [128, 16], F32); esel = sb("esel", [128, 1], F32)
        ugf = sb("ugf", [128, 33], F32); ugs = sb("ugs", [128, 33], F32); ugd = sb("ugd", [128, 33], F32)
        hb_ = [sb("hbr%d" % i, [128, 2048], BF16) for i in range(2)]; bhb = [Buf(), Buf()]
        b = Buf()
        OHs = c.OH[:, 0:nblk, :]
        for hf in range(2):
            b0, b1 = hf * (nblk // 2), (hf + 1) * (nblk // 2)
            n16 = (b1 - b0) * 16
            OHf = c.OH[:, b0:b1, :].rearrange("p a g -> p (a g)")
            p1, bp1 = c.ps[2 * hf], c.bps[2 * hf]
            K.mm(p1[:, 0:n16], c.ltri32[:], OHf, True, True, [c.bOH, c.bconst], [bp1])
            K.cp(R[:, b0:b1, :].rearrange("p a g -> p (a g)"), p1[:, 0:n16], [bp1], [b])
            p2, bp2 = c.ps[2 * hf + 1], c.bps[2 * hf + 1]
            K.mm(p2[:, 0:n16], c.ones32[:], OHf, True, True, [c.bOH, c.bc2], [bp2])
            K.cp(Bc[:, b0:b1, :].rearrange("p a g -> p (a g)"), p2[:, 0:n16], [bp2], [b])
        K.memset(run[:, 0, :], 0.0, [b])
        for blk in range(nblk):
            K.tt(run[:, blk + 1, :], run[:, blk, :], Bc[:, blk, :], ALU.add, [b], [b])
        K.memset(nun[:], 0.0, [b])
        for m in range(9):
            K.stt(nun[:], run[:, nblk, :], float(512 * m), nun[:], ALU.is_gt, ALU.add, [b], [b])
        K.memset(off[:], 0.0, [b])
        for e in range(1, 16):
            K.stt(off[:, e:e + 1], nun[:, e - 1:e], 512.0, off[:, e - 1:e], ALU.mult, ALU.add, [b], [b])
        K.tt(val[:, 0:nblk, :], R[:, 0:nblk, :], run[:, 0:nblk, :], ALU.add, [b], [b])
        K.tt(val[:, 0:nblk, :], val[:, 0:nblk, :], off[:].unsqueeze(1).to_broadcast([128, nblk, 16]), ALU.add, [b], [b])
        K.tt(valm[:, 0:nblk, :], val[:, 0:nblk, :], OHs, ALU.mult, [b, c.bOH], [b])
        K.red(pB[:, 0:nblk], valm[:, 0:nblk, :], ALU.max, [b], [b])
        K.stt(valm[:, 0:nblk, :], OHs, -1.0e6, val[:, 0:nblk, :], ALU.mult, ALU.add, [b, c.bOH], [b])
        K.ts(valm[:, 0:nblk, :], valm[:, 0:nblk, :], 1.0e6, None, ALU.add, None, [b], [b])
        K.red(pA[:, 0:nblk], valm[:, 0:nblk, :], ALU.min, [b], [b])
        K.tt(isA[:, 0:nblk, :], valm[:, 0:nblk, :], pA[:, 0:nblk].unsqueeze(2).to_broadcast([128, nblk, 16]), ALU.is_equal, [b], [b])
        K.tt(isA[:, 0:nblk, :], isA[:, 0:nblk, :], c.GS[:, 0:nblk, :], ALU.mult, [b, c.bGS], [b])
        K.red(gA[:, 0:nblk], isA[:, 0:nblk, :], ALU.add, [b], [b])
        K.red(gT_[:, 0:nblk], c.GS[:, 0:nblk, :], ALU.add, [b, c.bGS], [b])
        K.memset(c.GSA[:], 0.0, [c.bPOS]); K.memset(c.GSB[:], 0.0, [c.bPOS])
        K.cp(c.GSA[:, 0:nblk, 0], gA[:, 0:nblk], [b], [c.bPOS], part=True)
        K.tt(c.GSB[:, 0:nblk, 0], gT_[:, 0:nblk], gA[:, 0:nblk], ALU.subtract, [b], [c.bPOS], part=True)
        K.ts(pA[:, 0:nblk], pA[:, 0:nblk], float(NU * 512 - 1), 0.0, ALU.min, ALU.max, [b], [b])
        K.ts(pB[:, 0:nblk], pB[:, 0:nblk], float(NU * 512 - 1), 0.0, ALU.min, ALU.max, [b], [b])
        K.cp(c.POSA[:, 0:nblk], pA[:, 0:nblk], [b], [c.bPOS], part=True)
        K.cp(c.POSB[:, 0:nblk], pB[:, 0:nblk], [b], [c.bPOS], part=True)
        for u in range(NU):
            K.ts(cmp3[:, 0:15], off[:, 1:16], float(512 * u), None, ALU.is_le, None, [b], [b])
            K.red(esel[:], cmp3[:, 0:15], ALU.add, [b], [b])
            K.ts(ugf[:, u:u + 1], esel[:], float(16 * l), float(16 * l + 15), ALU.add, ALU.min, [b], [b])
        K.ts(ugs[:, 0:NU], ugf[:, 0:NU], 4096.0, None, ALU.mult, None, [b], [b])
        K.ts(ugd[:, 0:NU], ugf[:, 0:NU], 1024.0, None, ALU.mult, None, [b], [b])
        for u in range(NU):
            K.ts(c.IDXG[:, u, :], c.tgt[:, 0:32], ugs[:, u:u + 1], None, ALU.add, None, [b, c.bconst], [c.bIDX], part=True)
            K.ts(c.IDXD[:, u, :], c.tgt[:, 32:40], ugd[:, u:u + 1], None, ALU.add, None, [b, c.bconst], [c.bIDX], part=True)
        if c.dbg and l == 0:
            K.dma("sp", c.dposa, c.POSA[:], [c.bPOS], [Buf()]); K.dma("sp", c.dposb, c.POSB[:], [c.bPOS], [Buf()])
            K.dma("sp", c.didxg, c.IDXG[:].rearrange("p a b -> p (a b)"), [c.bIDX], [Buf()])
            K.dma("sp", c.didxd, c.IDXD[:].rearrange("p a b -> p (a b)"), [c.bIDX], [Buf()])
            K.dma("sp", c.dgsa, c.GSA[:].rearrange("p a b -> p (a b)"), [c.bPOS], [Buf()])
            K.dma("sp", c.doh, c.OH[:].rearrange("p a b -> p (a b)"), [c.bOH], [Buf()])
        for blk in range(nblk):
            i = blk % 2
            K.dma("sp", hb_[i][:], c.h2tok[blk * 128:(blk + 1) * 128, :], [c.bh2tok], [bhb[i]])
            for (POS_, GS_) in ((c.POSA, c.GSA), (c.POSB, c.GSB)):
                idx = POS_[:, blk:blk + 1]
                S.op("pool", lambda e, i=i, idx=idx: e.indirect_dma_start(out=c.h2sorted[:, :], out_offset=bass.IndirectOffsetOnAxis(ap=idx, axis=0),
                                                                          in_=hb_[i][:, :], in_offset=None),
                     [bhb[i], c.bPOS, c.bzero], [c.bh2s], dma=True, part=True)
                gs_ap = GS_[:, blk, :]
                S.op("pool", lambda e, gs_ap=gs_ap, idx=idx: e.indirect_dma_start(out=c.gsorted[:, :], out_offset=bass.IndirectOffsetOnAxis(ap=idx, axis=0),
                                                                                  in_=gs_ap, in_offset=None),
                     [c.bPOS, c.bzero], [c.bgs], dma=True, part=True)
        S.barrier()


def phase_moe(C, l):
    c = NS(C)
    nc, S, K, PP = c.nc, c.S, c.K, c.PP
    NU = 33 if l == 0 else 31
    with contextlib.ExitStack() as ph:
        def sb(n, s, d):
            return ph.enter_context(SBT(nc, n, s, d))
        hs = sb("hs0", [128, 4, 2048], BF16); bhs = Buf()
        gsl = [sb("gsl%d" % i, [128, 4, 4], F32) for i in range(2)]; bgsl = [Buf(), Buf()]
        hT = sb("hT", [128, 16, 512], BF16); bhT = Buf()
        ae = sb("ae", [128, 8, 512], BF16); bae = Buf()
        acc = sb("acc", [128, 4, 2048], BF16); bacc = [Buf() for _ in range(4)]
        wgs = [sb("wgs%d" % i, [128, 16, 512], BF16) for i in range(2)]; bwg = [Buf(), Buf()]
        wus = [sb("wus%d" % i, [128, 16, 512], BF16) for i in range(2)]; bwu = [Buf(), Buf()]
        wds1 = sb("wds1", [128, 8, 2048], BF16); bwd1 = Buf()
        sil = [sb("sil%d" % i, [128, 512], F32) for i in range(2)]; bsil = [Buf(), Buf()]
        wgv = c.wg.rearrange("e k (h n) -> (e k h) n", h=2)
        wuv = c.wu.rearrange("e k (h n) -> (e k h) n", h=2)
        wdv = c.wd.rearrange("e f n -> (e f) n")
        st_ = dict(wi=0, si=0)

        def gat(dst, src2d, idx, wbuf):
            nrow = src2d.shape[0]
            S.op("pool", lambda e: e.indirect_dma_start(out=dst, out_offset=None, in_=src2d, in_offset=bass.IndirectOffsetOnAxis(ap=idx, axis=0)),
                 [c.bIDX], [wbuf], dma=True, part=True)

        def load(u):
            rows = slice(u * 512, (u + 1) * 512)
            K.dma("sp", hs[:], c.h2sorted[rows, :].rearrange("(a p) d -> p a d", p=128), [c.bh2s], [bhs])
            K.dma("sp", gsl[u % 2][:], c.gsorted[rows, :].rearrange("(a p) e -> p a e", p=128), [c.bgs], [bgsl[u % 2]])

        def transposes(u):
            for sbk in range(4):
                for hf in range(2):
                    p_, bp_ = PP.next(0, 4)
                    pv = p_[:].bitcast(BF16)
                    for kk in range(8):
                        k = hf * 8 + kk
                        K.tr(pv[:, kk * 128:(kk + 1) * 128], hs[:, sbk, k * 128:(k + 1) * 128], c.idb[:], [bhs, c.bc2], [bp_], part=True)
                    K.cp(hT[:, hf * 8:(hf + 1) * 8, sbk * 128:(sbk + 1) * 128], pv[:, 0:1024].rearrange("p (k t) -> p k t", k=8), [bp_], [bhT], part=True)

        def gateup(u):
            for hf in range(2):
                w_ = st_["wi"] % 2; st_["wi"] += 1
                for k in range(16):
                    gat(wgs[w_][:, k, :], wgv[:, :], c.IDXG[:, u, 2 * k + hf:2 * k + hf + 1], bwg[w_])
                for k in range(16):
                    gat(wus[w_][:, k, :], wuv[:, :], c.IDXG[:, u, 2 * k + hf:2 * k + hf + 1], bwu[w_])
                for jj in range(4):
                    j = hf * 4 + jj
                    pg, bpg = PP.next(0, 4)
                    for k in range(16):
                        K.mm(pg[:], wgs[w_][:, k, jj * 128:(jj + 1) * 128], hT[:, k, :], k == 0, k == 15, [bwg[w_], bhT], [bpg])
                    pu, bpu = PP.next(0, 4)
                    for k in range(16):
                        K.mm(pu[:], wus[w_][:, k, jj * 128:(jj + 1) * 128], hT[:, k, :], k == 0, k == 15, [bwu[w_], bhT], [bpu])
                    s_ = st_["si"] % 2; st_["si"] += 1
                    K.act(sil[s_][:], pg[:], AF.Silu, [bpg], [bsil[s_]])
                    K.tt(ae[:, j, :], sil[s_][:], pu[:], ALU.mult, [bsil[s_], bpu], [bae], part=True)
            for j in range(8):
                gat(wds1[:, j, :], wdv[:, :], c.IDXD[:, u, j:j + 1], bwd1)

        def down(u):
            i = u % 2
            for sbk in range(4):
                for dg in range(4):
                    po, bpo = PP.next(4, 8)
                    dc = slice(dg * 512, (dg + 1) * 512)
                    for j in range(8):
                        K.mm(po[:], ae[:, j, sbk * 128:(sbk + 1) * 128], wds1[:, j, dc], j == 0, j == 7, [bwd1, bae], [bpo])
                    K.ts(acc[:, sbk, dc], po[:], gsl[i][:, sbk, 0:1], None, ALU.mult, None, [bpo, bgsl[i]], [bacc[sbk]], part=True)
                r0 = u * 512 + sbk * 128
                K.dma("sp", c.fsorted[r0:r0 + 128, :], acc[:, sbk, :], [bacc[sbk]], [c.bfs], part=True)
        load(0)
        transposes(0)
        for u in range(NU):
            if u + 1 < NU:
                load(u + 1)
            gateup(u)
            if u + 1 < NU:
                transposes(u + 1)
            down(u)
        S.barrier()


def phase_ln2(C, l):
    c = NS(C)
    nc, S, K, PP = c.nc, c.S, c.K, c.PP
    ntile = 9 if l == 0 else 8
    with contextlib.ExitStack() as ph:
        def sb(n, s, d):
            return ph.enter_context(SBT(nc, n, s, d))
        x32 = [sb("x32f%d" % i, [128, 16, 512], F32) for i in range(2)]; bx = [[Buf() for _ in range(16)] for _ in range(2)]
        ftAs = [sb("ftkA%d" % i, [128, 4, 2048], BF16) for i in range(2)]; ftBs = [sb("ftkB%d" % i, [128, 4, 2048], BF16) for i in range(2)]
        bfAs = [Buf(), Buf()]; bfBs = [Buf(), Buf()]
        zsq = [sb("zsqf%d" % i, [128, 512], F32) for i in range(2)]; bzsq = [Buf(), Buf()]
        mean = sb("meanf", [128, 512], F32); msq = sb("msqf", [128, 512], F32); var = sb("varf", [128, 512], F32); rstd = sb("rstdf", [128, 512], F32)
        bst = Buf()
        def stage1(tt):
            lat = tt < 8
            j = tt // 4 if lat else 2
            cols = slice(tt * 512, (tt + 1) * 512)
            i = tt % 2
            for hk in range(2):
                K.dma("sp", x32[i][:, hk * 8:(hk + 1) * 8, :], c.xs[hk * 1024:(hk + 1) * 1024, cols].rearrange("(c p) t -> p c t", p=128), [c.bxs[tt]], bx[i][hk * 8:(hk + 1) * 8])
            ftA, ftB, bfA, bfB = ftAs[i], ftBs[i], bfAs[i], bfBs[i]
            for tb in range(4):
                for (POS_, ft_, bf_) in ((c.POSA, ftA, bfA), (c.POSB, ftB, bfB)):
                    idx = POS_[:, tt * 4 + tb:tt * 4 + tb + 1]
                    dst = ft_[:, tb, :]
                    S.op("pool", lambda e, idx=idx, dst=dst: e.indirect_dma_start(out=dst, out_offset=None, in_=c.fsorted[:, :],
                                                                                  in_offset=bass.IndirectOffsetOnAxis(ap=idx, axis=0)),
                         [c.bfs, c.bPOS], [bf_], dma=True, part=True)
            for k in range(16):
                p_, bp_ = PP.next(0, 6)
                for tb in range(4):
                    K.mm(p_[:, tb * 128:(tb + 1) * 128], ftA[:, tb, k * 128:(k + 1) * 128], c.idb[:], True, False, [bfA, c.bc2], [bp_], part=True)
                    K.mm(p_[:, tb * 128:(tb + 1) * 128], ftB[:, tb, k * 128:(k + 1) * 128], c.idb[:], False, True, [bfB, c.bc2], [bp_], part=True)
                K.stt(x32[i][:, k, :], p_[:], c.modT[:, l, 80 + k, j:j + 1], x32[i][:, k, :], ALU.mult, ALU.add, [bp_, bx[i][k], c.bmod], [bx[i][k]])

        def stage2(tt):
            cols = slice(tt * 512, (tt + 1) * 512)
            i = tt % 2

            def post(k, i=i):
                K.act(x32[i][:, k, :], x32[i][:, k, :], AF.Identity, [bx[i][k], c.bconst], [bx[i][k]], bias=c.lnt[:, l, 3, k:k + 1], scale=c.lnt[:, l, 2, k:k + 1])
            ln_tile(c, K, x32[i], bx[i], None, None, LN_EPS, zsq, bzsq, mean, msq, var, rstd, bst, post)
            if l == 0:
                K.dma("sp", c.xs[:, cols].rearrange("(c p) t -> p c t", p=128), x32[i][:], bx[i], [c.bxs[tt]])
            else:
                K.dma("sp", c.out[:, cols].rearrange("(c p) t -> p c t", p=128), x32[i][:], bx[i], [c.bout], part=True)
        stage1(0)
        for tt in range(ntile):
            if tt + 1 < ntile:
                stage1(tt + 1)
            stage2(tt)
        S.barrier()


def _host_prep(inp):
    f = lambda a: np.ascontiguousarray(np.asarray(a, dtype=np.float32))
    sh = {}
    sh["w_ada"] = f(inp["w_ada"])
    sh["bada"] = f(np.asarray(inp["b_ada"]).reshape(2, 96, 128).transpose(2, 0, 1))
    ln = np.stack([np.asarray(inp["ln1_g"]), np.asarray(inp["ln1_b"]), np.asarray(inp["ln2_g"]), np.asarray(inp["ln2_b"])], 1)
    sh["lnp"] = f(ln.reshape(2, 4, 16, 128).transpose(3, 0, 1, 2))
    sh["w_in0"] = f(np.asarray(inp["w_in_even"])[0]); sh["w_in1"] = f(np.asarray(inp["w_in_odd"])[0])
    sh["w_out0"] = f(np.asarray(inp["w_out_even"])[0]); sh["w_out1"] = f(np.asarray(inp["w_out_odd"])[0])
    sh["sinkb"] = f(np.broadcast_to(np.asarray(inp["sink_logits"])[0][None, :], (128, 8)))
    rpb = np.asarray(inp["na_rpb"])[0]
    col = np.arange(64)
    dcol = np.clip(col[None, :] - col[:, None] + 15, 0, 30)
    sh["rpbx"] = f(rpb[:, :, dcol].transpose(3, 0, 1, 2))
    cs = np.clip(col - 8, 0, 48)
    col_ok = (col[None, :] >= cs[:, None]) & (col[None, :] < cs[:, None] + 16)
    sh["cmask"] = f(col_ok.T.astype(np.float32))
    sh["qkg"] = f(np.stack([np.asarray(inp["q_norm_g"])[0], np.asarray(inp["k_norm_g"])[0]], 1))
    sh["lamv"] = f(np.stack([np.asarray(inp[k])[0] for k in ("lambda_q1", "lambda_k1", "lambda_q2", "lambda_k2")], 1))
    sh["subln"] = f(np.asarray(inp["subln_g"])[0].reshape(2, 128).T)
    sh["wr"] = f(np.asarray(inp["w_router"]).reshape(16, 128, 16).transpose(1, 0, 2))
    sh["rbias"] = f(np.broadcast_to(np.tile(np.asarray(inp["router_bias"]), 4)[None, :], (128, 64)))
    sh["wg"] = f(inp["w_exp_gate"]).reshape(32, D, 1024); sh["wu"] = f(inp["w_exp_up"]).reshape(32, D, 1024); sh["wd"] = f(inp["w_exp_down"]).reshape(32, 1024, D)
    t = np.arange(2048)
    row = (t // 64).astype(np.float32); colt = (t % 64).astype(np.float32)
    inv = (np.float32(10000.0) ** (-np.arange(32, dtype=np.float32) / np.float32(32))).astype(np.float32)
    ang = np.concatenate([row[:, None] * inv[None], colt[:, None] * inv[None]], -1).astype(np.float32)
    cosv = np.cos(ang).astype(np.float32); sinv = np.sin(ang).astype(np.float32)
    sh["ropec"] = f(np.repeat(cosv.T, 2, axis=0)); sh["ropes"] = f(np.repeat(sinv.T, 2, axis=0))
    pm = np.zeros((128, 128), np.float32)
    for i in range(64):
        pm[2 * i + 1, 2 * i] = -1.0
        pm[2 * i, 2 * i + 1] = 1.0
    sh["perm"] = pm
    sh["ident"] = np.eye(128, dtype=np.float32)
    a = np.arange(128)
    tr = np.zeros((128, 2, 4, 128), np.float32)
    tr[:, 0] = (a[None, :] <= a[:, None]).astype(np.float32)[:, None, :]
    tr[:, 1] = (a[:, None] <= a[None, :]).astype(np.float32)[:, None, :]
    sh["trim"] = tr
    sh["ltri"] = (a[:, None] < a[None, :]).astype(np.float32)
    tg = np.zeros((128, 48), np.float32)
    for cc in range(16):
        for hh in range(2):
            tg[:, cc * 2 + hh] = 2 * (cc * 128 + a) + hh
    for jj in range(8):
        tg[:, 32 + jj] = jj * 128 + a
    sh["tgd"] = tg
    x = np.asarray(inp["x"]); ctx = np.asarray(inp["ctx"]); cc = np.asarray(inp["c"]); c_ctx = np.asarray(inp["c_ctx"])
    maps = []
    for i in range(NCORES):
        m = dict(sh)
        m["xin"] = f(np.concatenate([x[2 * i].T, x[2 * i + 1].T, ctx[2 * i].T, ctx[2 * i + 1].T], axis=1))
        m["cin"] = f(np.stack([cc[2 * i], cc[2 * i + 1], c_ctx], 0).reshape(3, 16, 128).transpose(2, 1, 0))
        maps.append(m)
    return maps


_NC_CACHE = {}


def kernel(**inputs):
    maps = _host_prep(inputs)
    if "nc" not in _NC_CACHE:
        _NC_CACHE["nc"] = build()
    nc = _NC_CACHE["nc"]
    res = run_bass_kernel_spmd(nc, maps, core_ids=list(range(NCORES)))
    y = np.empty((16, 2048, 2048), np.float32)
    for i in range(NCORES):
        o = np.asarray(res.results[i]["out"])
        y[2 * i] = o[:, 0:2048].T
        y[2 * i + 1] = o[:, 2048:4096].T
    return y
```

```python
import math
import contextlib
import numpy as np
import concourse.bass as bass
import concourse.mybir as mybir
from concourse.bass_utils import run_bass_kernel_spmd

F32 = mybir.dt.float32
BF16 = mybir.dt.bfloat16
AF = mybir.ActivationFunctionType
ALU = mybir.AluOpType
AX = mybir.AxisListType

NCORES = 8
D = 2048
NTOK = 4608
NLAT = 4096
ALPHA = 4.0 ** 0.25
LN_EPS = 1e-5 / (ALPHA * ALPHA)
SCALE = 128.0 ** -0.5
LAMBDA_INIT1 = 0.8 - 0.6 * math.exp(-0.3 * 1)
BIG = 1.0e4


class Buf:
    __slots__ = ("w", "r", "dkey", "epoch")

    def __init__(self):
        self.w = None
        self.r = {}
        self.dkey = None
        self.epoch = -1


class Sched:
    ENGS = ("pe", "act", "dve", "pool", "sp")

    def __init__(self, nc):
        self.nc = nc
        self.q = {e: [] for e in self.ENGS}
        self.cnt = {}
        self.seen = {e: {} for e in self.ENGS}
        self.epoch = 0
        self.free = []
        self.nd = 0

    def _dkey(self, b):
        if b.dkey is None or b.epoch != self.epoch:
            if self.free:
                b.dkey = self.free.pop()
            else:
                b.dkey = ("d", self.nd)
                self.nd += 1
            b.epoch = self.epoch
            self.live.append(b.dkey)
        return b.dkey

    live = None

    def op(self, eng, fn, reads=(), writes=(), dma=False, part=False):
        if self.live is None:
            self.live = []
        waits = {}
        seen = self.seen[eng]

        def need(tok):
            if tok is None:
                return
            k, v = tok
            if k == "pe" and eng == "pe":
                return
            if seen.get(k, 0) >= v:
                return
            if waits.get(k, 0) < v:
                waits[k] = v

        for b in reads:
            need(b.w)
        for i, b in enumerate(writes):
            if not (part and i == 0):
                need(b.w)
            for k, v in b.r.items():
                need((k, v))
        for k, v in waits.items():
            seen[k] = v
        if dma:
            key, amt = self._dkey(writes[0]), 16
        else:
            key, amt = eng, 1
        val = self.cnt.get(key, 0) + amt
        self.cnt[key] = val
        tok = (key, val)
        for b in reads:
            if b.r.get(key, 0) < val:
                b.r[key] = val
        for b in writes:
            b.w = tok
            b.r = {}
        self.q[eng].append((tuple(waits.items()), fn, key, amt))
        return tok

    def barrier(self):
        for e in self.ENGS:
            seen = self.seen[e]
            waits = {}
            for k, v in self.cnt.items():
                if seen.get(k, 0) < v and not (k == e and e == "pe"):
                    waits[k] = v
                    seen[k] = v
            self.q[e].append((tuple(waits.items()), None, None, 0))
        if self.live:
            self.free.extend(self.live)
            self.live = []
        self.epoch += 1

    def emit(self):
        nc = self.nc
        sems = {}
        with contextlib.ExitStack() as st:
            for k in self.cnt.keys():
                nm = "s_" + (k if isinstance(k, str) else "d%d" % k[1])
                sems[k] = st.enter_context(nc.semaphore(nm))
            block = st.enter_context(nc.Block())
            engmap = {"pe": block.tensor, "act": block.scalar, "dve": block.vector,
                      "pool": block.gpsimd, "sp": block.sync}
            for en in self.ENGS:
                lst = self.q[en]

                def body(e, lst=lst):
                    for waits, fn, key, amt in lst:
                        for k, v in waits:
                            e.wait_ge(sems[k], v)
                        if fn is not None:
                            fn(e).then_inc(sems[key], amt)
                engmap[en](body)


class KB:
    def __init__(self, S):
        self.S = S

    def dma(self, q, out, in_, reads, writes, part=False):
        return self.S.op(q, lambda e: e.dma_start(out=out, in_=in_), reads, writes, dma=True, part=part)

    def mm(self, out, lhsT, rhs, start, stop, reads, writes, part=False):
        return self.S.op("pe", lambda e: e.matmul(out, lhsT=lhsT, rhs=rhs, start=start, stop=stop), reads, writes, part=part)

    def tr(self, out, in_, ident, reads, writes, part=False):
        return self.S.op("pe", lambda e: e.transpose(out, in_, ident), reads, writes, part=part)

    def act(self, out, in_, func, reads, writes, bias=None, scale=None, part=False):
        kw = {}
        if bias is not None:
            kw["bias"] = bias
        if scale is not None:
            kw["scale"] = scale
        return self.S.op("act", lambda e: e.activation(out=out, in_=in_, func=func, **kw), reads, writes, part=part)

    def ts(self, out, in0, s1, s2, op0, op1, reads, writes, eng="dve", part=False):
        if op1 is None:
            return self.S.op(eng, lambda e: e.tensor_scalar(out=out, in0=in0, scalar1=s1, scalar2=None, op0=op0), reads, writes, part=part)
        return self.S.op(eng, lambda e: e.tensor_scalar(out=out, in0=in0, scalar1=s1, scalar2=s2, op0=op0, op1=op1), reads, writes, part=part)

    def tt(self, out, in0, in1, op, reads, writes, eng="dve", part=False):
        return self.S.op(eng, lambda e: e.tensor_tensor(out=out, in0=in0, in1=in1, op=op), reads, writes, part=part)

    def stt(self, out, in0, scalar, in1, op0, op1, reads, writes, eng="dve", part=False):
        return self.S.op(eng, lambda e: e.scalar_tensor_tensor(out=out, in0=in0, scalar=scalar, in1=in1, op0=op0, op1=op1), reads, writes, part=part)

    def cp(self, out, in_, reads, writes, eng="dve", part=False):
        return self.S.op(eng, lambda e: e.tensor_copy(out=out, in_=in_), reads, writes, part=part)

    def red(self, out, in_, op, reads, writes, part=False):
        return self.S.op("dve", lambda e: e.tensor_reduce(out=out, in_=in_, axis=AX.X, op=op), reads, writes, part=part)

    def recip(self, out, in_, reads, writes, part=False):
        return self.S.op("dve", lambda e: e.reciprocal(out=out, in_=in_), reads, writes, part=part)

    def rsqrt(self, out, in_, eps, reads, writes):
        self.ts(out, in_, eps, None, ALU.add, None, reads, writes)
        self.act(out, out, AF.Sqrt, list(writes), writes)
        return self.recip(out, out, list(writes), writes)

    def memset(self, out, val, writes, eng="dve"):
        return self.S.op(eng, lambda e: e.memset(out, val), (), writes)


def build(dbg=False, stop=99):
    nc = bass.Bass("TRN2", target_bir_lowering=False)
    S = Sched(nc)
    S.live = []
    K = KB(S)

    def din(name, shape, dt=F32):
        return nc.dram_tensor(name, list(shape), dt, kind="ExternalInput").ap()
    skind = "ExternalOutput" if dbg else "Internal"

    def dscr(name, shape, dt):
        return nc.dram_tensor(name, list(shape), dt, kind=skind).ap()

    xin = din("xin", [D, NTOK]); cin = din("cin", [128, 16, 3]); w_ada = din("w_ada", [2, D, 6 * D])
    bada = din("bada", [128, 2, 96]); lnp = din("lnp", [128, 2, 4, 16])
    w_in = [din("w_in0", [D, 4608]), din("w_in1", [D, 4608])]
    w_out = [din("w_out0", [D, D]), din("w_out1", [D, D])]
    sinkb = din("sinkb", [128, 8]); rpbx = din("rpbx", [64, 8, 15, 64]); cmask = din("cmask", [64, 64])
    qkg = din("qkg", [128, 2]); lamv = din("lamv", [128, 4]); subln = din("subln", [128, 2])
    wr = din("wr", [128, 16, 16]); rbias = din("rbias", [128, 64])
    wg = din("wg", [32, D, 1024]); wu = din("wu", [32, D, 1024]); wd = din("wd", [32, 1024, D])
    ropec = din("ropec", [128, 2048]); ropes = din("ropes", [128, 2048])
    perm = din("perm", [128, 128]); ident = din("ident", [128, 128]); trim = din("trim", [128, 2, 4, 128])
    out = nc.dram_tensor("out", [D, NLAT], F32, kind="ExternalOutput").ap()
    xs = dscr("xs", [D, NTOK], F32); fmn = dscr("fmn", [26 * 128, NTOK], BF16); fmr = dscr("fmr", [26 * 128, NLAT], BF16)
    vtok = dscr("vtok", [NTOK, 1280], BF16); oT = dscr("oT", [D, NTOK], BF16); h2T = dscr("h2T", [D, NTOK], BF16)
    gT = dscr("gT", [16, NTOK], F32); fT = dscr("fT", [D, NTOK], F32)
    NSLOT = 33 * 512
    h2tok = dscr("h2tok", [NTOK, D], BF16); h2sorted = dscr("h2sorted", [NSLOT, D], BF16)
    gsorted = dscr("gsorted", [NSLOT, 4], F32); fsorted = dscr("fsorted", [NSLOT, D], BF16)
    ltri = din("ltri", [128, 128]); tgd = din("tgd", [128, 48])
    bh2tok = Buf(); bh2s = Buf(); bgs = Buf(); bfs = Buf()
    bxs = [Buf() for _ in range(9)]
    bfmn = Buf(); bfmr = Buf(); bvtok = Buf(); boT = Buf(); bh2T = Buf(); bgT = Buf(); bfT = Buf(); bout = Buf()

    with contextlib.ExitStack() as top:
        def gsb(n, s, d):
            return top.enter_context(SBT(nc, n, s, d))
        ps = [top.enter_context(nc.psum_tensor("ps%d" % i, [128, 512], F32)) for i in range(8)]
        bps = [Buf() for _ in range(8)]

        class PP:
            i = 0

            @classmethod
            def next(cls, lo=0, hi=8):
                n = hi - lo
                k = lo + (cls.i % n)
                cls.i += 1
                return ps[k], bps[k]

        ones32 = gsb("ones32", [128, 128], F32); onesb = gsb("onesb", [128, 128], BF16)
        perm32 = gsb("perm32", [128, 128], F32); id32 = gsb("id32", [128, 128], F32)
        modT = gsb("modT", [128, 2, 96, 3], F32); lnt = gsb("lnt", [128, 2, 4, 16], F32)
        badat = gsb("badat", [128, 2, 96], F32)
        cint = gsb("cint", [128, 16, 3], F32); cbf = gsb("cbf", [128, 16, 3], BF16)
        qkgt = gsb("qkgt", [128, 2], F32); lamt = gsb("lamt", [128, 8], F32); sublnt = gsb("sublnt", [128, 2], F32)
        wrt = gsb("wrt", [128, 16, 16], F32); rbt = gsb("rbt", [128, 64], F32)
        sinkt = gsb("sinkt", [128, 8], F32)
        ltri32 = gsb("ltri32", [128, 128], F32); idb = gsb("idb", [128, 128], BF16)
        OH = gsb("OH", [128, 36, 16], F32); GS = gsb("GS", [128, 36, 16], F32)
        POSA = gsb("POSA", [128, 36], mybir.dt.int32); POSB = gsb("POSB", [128, 36], mybir.dt.int32)
        GSA = gsb("GSA", [128, 36, 4], F32); GSB = gsb("GSB", [128, 36, 4], F32)
        bOH = Buf(); bGS = Buf(); bPOS = Buf(); bUG = Buf(); bzero = Buf()
        tgt = gsb("tgt", [128, 48], F32)
        IDXG = gsb("IDXG", [128, 33, 32], mybir.dt.int32); IDXD = gsb("IDXD", [128, 33, 8], mybir.dt.int32); bIDX = Buf()
        bconst = Buf(); bmod = Buf(); bc2 = Buf()
        K.memset(ones32[:], 1.0, [bc2]); K.memset(onesb[:], 1.0, [bc2])
        for (t_, a_) in ((perm32[:], perm), (id32[:], ident), (lnt[:], lnp), (badat[:], bada), (cint[:], cin), (qkgt[:], qkg),
                         (lamt[:, 0:4], lamv), (sublnt[:], subln), (wrt[:], wr), (rbt[:], rbias), (sinkt[:], sinkb), (ltri32[:], ltri), (tgt[:], tgd)):
            K.dma("sp", t_, a_, [], [bconst], part=True)
        K.cp(idb[:], id32[:], [bconst], [bc2])
        K.act(cbf[:], cint[:], AF.Silu, [bconst], [bc2])
        K.ts(qkgt[:], qkgt[:], math.sqrt(128.0), None, ALU.mult, None, [bconst], [bconst])
        K.ts(sublnt[:], sublnt[:], 16.0 * (1.0 - LAMBDA_INIT1), None, ALU.mult, None, [bconst], [bconst])
        K.act(sinkt[:], sinkt[:], AF.Exp, [bconst], [bconst])
        K.tt(lamt[:, 4:5], lamt[:, 0:1], lamt[:, 1:2], ALU.mult, [bconst], [bconst])
        K.tt(lamt[:, 5:6], lamt[:, 2:3], lamt[:, 3:4], ALU.mult, [bconst], [bconst])
        p_, bp_ = PP.next()
        K.mm(p_[:, 0:2], ones32[:], lamt[:, 4:6], True, True, [bconst, bc2], [bp_])
        K.act(lamt[:, 4:6], p_[:, 0:2], AF.Exp, [bp_], [bconst])
        K.tt(lamt[:, 6:7], lamt[:, 5:6], lamt[:, 4:5], ALU.subtract, [bconst], [bconst])
        K.ts(lamt[:, 6:7], lamt[:, 6:7], -LAMBDA_INIT1, None, ALU.add, None, [bconst], [bconst])

        with contextlib.ExitStack() as ph:
            wsl = [ph.enter_context(SBT(nc, "wa%d" % i, [128, 16, 512], BF16)) for i in range(2)]
            bwsl = [Buf(), Buf()]
            si = 0
            zt = ph.enter_context(SBT(nc, "zt", [128, 2048], BF16)); bz = Buf()
            K.memset(zt[:], 0.0, [bz])
            for i in range(33 * 4):
                K.dma("sp", h2sorted[i * 128:(i + 1) * 128, :], zt[:], [bz], [bzero], part=True)
            for l in range(1):
                for s in range(24):
                    w_ = si % 2; si += 1
                    K.dma("pool", wsl[w_][:], w_ada[l, :, s * 512:(s + 1) * 512].rearrange("(c p) n -> p c n", p=128), [], [bwsl[w_]])
                    p_, bp_ = PP.next()
                    for oc in range(4):
                        for c in range(16):
                            K.mm(p_[:, oc * 3:(oc + 1) * 3], wsl[w_][:, c, oc * 128:(oc + 1) * 128], cbf[:, c, :], c == 0, c == 15, [bwsl[w_], bc2], [bp_])
                    for j in range(3):
                        K.tt(modT[:, l, 4 * s:4 * s + 4, j], p_[:, j:12:3], badat[:, l, 4 * s:4 * s + 4], ALU.add, [bp_, bconst], [bmod], )
                K.ts(modT[:, l, 16:32, :], modT[:, l, 16:32, :], 1.0, None, ALU.add, None, [bmod], [bmod])
                K.ts(modT[:, l, 64:80, :], modT[:, l, 64:80, :], 1.0, None, ALU.add, None, [bmod], [bmod])
                K.ts(modT[:, l, 32:48, :], modT[:, l, 32:48, :], 1.0 / ALPHA, None, ALU.mult, None, [bmod], [bmod])
                K.ts(modT[:, l, 80:96, :], modT[:, l, 80:96, :], 1.0 / ALPHA, None, ALU.mult, None, [bmod], [bmod])
            S.barrier()
        if dbg:
            dmod = nc.dram_tensor("dmod", [128, 2 * 96 * 3], F32, kind="ExternalOutput").ap()
            K.dma("sp", dmod, modT[:].rearrange("p a b c -> p (a b c)"), [bmod], [Buf()])
            dlam = nc.dram_tensor("dlam", [128, 8], F32, kind="ExternalOutput").ap()
            K.dma("sp", dlam, lamt[:], [bconst], [Buf()])
            S.barrier()
        if dbg:
            I32_ = mybir.dt.int32
            dposa = nc.dram_tensor("dposa", [128, 36], I32_, kind="ExternalOutput").ap()
            dposb = nc.dram_tensor("dposb", [128, 36], I32_, kind="ExternalOutput").ap()
            didxg = nc.dram_tensor("didxg", [128, 33 * 32], I32_, kind="ExternalOutput").ap()
            didxd = nc.dram_tensor("didxd", [128, 33 * 8], I32_, kind="ExternalOutput").ap()
            dgsa = nc.dram_tensor("dgsa", [128, 36 * 4], F32, kind="ExternalOutput").ap()
            doh = nc.dram_tensor("doh", [128, 36 * 16], F32, kind="ExternalOutput").ap()
        C = dict(locals())
        for l in range(2):
            if stop <= 10 * l + 1:
                break
            phase_qkv(C, l)
            if stop <= 10 * l + 2:
                break
            phase_attn(C, l)
            if stop <= 10 * l + 3:
                break
            phase_oproj(C, l)
            phase_route(C, l)
            if stop <= 10 * l + 4:
                break
            phase_moe(C, l)
            if stop <= 10 * l + 5:
                break
            phase_ln2(C, l)
        S.barrier()
        S.emit()
    return nc


_UNIQ = [0]


def SBT(nc, name, shape, dt):
    _UNIQ[0] += 1
    return nc.sbuf_tensor("%s_u%d" % (name, _UNIQ[0]), shape, dt)


class NS:
    def __init__(self, d):
        self.__dict__.update(d)


def phase_qkv(C, l):
    c = NS(C)
    nc, S, K, PP = c.nc, c.S, c.K, c.PP
    src = c.xin if l == 0 else c.xs
    with contextlib.ExitStack() as ph:
        def sb(n, s, d):
            return ph.enter_context(SBT(nc, n, s, d))

        def pair(n, s, d, depth=2):
            return [sb("%s%d" % (n, i), s, d) for i in range(depth)], [Buf() for _ in range(depth)]
        QD = 4
        x32, bx32 = pair("x32_", [128, 16, 512], F32, 1)
        x32 = x32 * 2; bx32 = bx32 * 2
        hb, bhb = pair("hb_", [128, 16, 512], BF16)
        ws, bws = pair("ws_", [128, 16, 512], BF16)
        rc = sb("rc", [128, 2048], F32); rs_ = sb("rs", [128, 2048], F32); brc = Buf(); brs = Buf()
        q32, bq32 = pair("q32_", [128, 512], F32, QD)
        sq, bsq = pair("sq_", [128, 512], F32, QD)
        rr, brr = pair("rr_", [128, 512], F32, QD)
        t1, bt1 = pair("t1_", [128, 512], F32, QD)
        t2, bt2 = pair("t2_", [128, 512], F32, QD)
        stn, bstn = pair("stn_", [128, 512], BF16, QD)
        str_, bstr = pair("str_", [128, 512], BF16, QD)
        vst, bvst = pair("vst_", [128, 512], BF16)
        K.dma("sp", rc[:], c.ropec, [], [brc]); K.dma("sp", rs_[:], c.ropes, [], [brs])
        si = 0; qi = 0; vi = 0
        for tt in range(9):
            lat = tt < 8
            j = tt // 4 if lat else 2
            tok0 = (tt % 4) * 512
            xb = tt % 2
            cols = slice(tt * 512, (tt + 1) * 512)
            K.dma("sp", x32[xb][:], src[:, cols].rearrange("(c p) t -> p c t", p=128), [c.bxs[tt]], [bx32[xb]])
            for k in range(16):
                K.ts(hb[xb][:, k, :], x32[xb][:, k, :], c.modT[:, l, 16 + k, j:j + 1], c.modT[:, l, k, j:j + 1],
                     ALU.mult, ALU.add, [bx32[xb], c.bmod], [bhb[xb]], part=True)
            slabs = list(range(9)) if (lat or l == 0) else [2, 5, 6, 7, 8]
            for s in slabs:
                w_ = si % 2; si += 1
                K.dma("pool", ws[w_][:], c.w_in[l][:, s * 512:(s + 1) * 512].rearrange("(c p) n -> p c n", p=128), [], [bws[w_]])
                if s in (2, 7, 8):
                    c0, ncol = (256, 256) if s == 2 else (0, 512)
                    vcol = 0 if s == 2 else 256 + (s - 7) * 512
                    for tb in range(4):
                        p_, bp_ = PP.next()
                        for k in range(16):
                            K.mm(p_[:, 0:ncol], hb[xb][:, k, tb * 128:(tb + 1) * 128], ws[w_][:, k, c0:c0 + ncol], k == 0, k == 15,
                                 [bhb[xb], bws[w_]], [bp_])
                        v = vi % 2; vi += 1
                        K.act(vst[v][:, 0:ncol], p_[:, 0:ncol], AF.Copy, [bp_], [bvst[v]])
                        r0 = tt * 512 + tb * 128
                        K.dma("sp", c.vtok[r0:r0 + 128, vcol:vcol + ncol], vst[v][:, 0:ncol], [bvst[v]], [c.bvtok], part=True)
                fch = [0, 1, 2, 3] if s not in (2, 7, 8) else ([0, 1] if s == 2 else [])
                items = []
                for ci in fch:
                    co = s * 4 + ci
                    if l == 1 and (not lat) and (co < 8 or 12 <= co < 20):
                        continue
                    fi = co if co < 10 else co - 2
                    norm = (l == 1 and co < 10)
                    rope = lat and (co < 10 or l == 1)
                    nopos = (not lat) or co < 8 or (12 <= co < 20) or (l == 0 and co >= 20)
                    p_, bp_ = PP.next()
                    for k in range(16):
                        K.mm(p_[:], ws[w_][:, k, ci * 128:(ci + 1) * 128], hb[xb][:, k, :], k == 0, k == 15, [bhb[xb], bws[w_]], [bp_])
                    q = qi % QD; qi += 1
                    items.append(dict(co=co, fi=fi, norm=norm, rope=rope, nopos=nopos, p=p_, bp=bp_, q=q))
                for it in items:
                    q = it["q"]
                    K.act(q32[q][:], it["p"][:], AF.Copy, [it["bp"]], [bq32[q]])
                nl = [it for it in items if it["norm"]]
                for it in nl:
                    q = it["q"]
                    K.act(sq[q][:], q32[q][:], AF.Square, [bq32[q]], [bsq[q]])
                for it in nl:
                    q = it["q"]
                    it["p2"], it["bp2"] = PP.next()
                    K.mm(it["p2"][:], c.ones32[:], sq[q][:], True, True, [bsq[q], c.bc2], [it["bp2"]])
                for it in nl:
                    q = it["q"]
                    K.ts(rr[q][:], it["p2"][:], 128.0 * 1e-6, None, ALU.add, None, [it["bp2"]], [brr[q]])
                for it in nl:
                    q = it["q"]
                    K.act(rr[q][:], rr[q][:], AF.Sqrt, [brr[q]], [brr[q]])
                for it in nl:
                    q = it["q"]
                    K.recip(rr[q][:], rr[q][:], [brr[q]], [brr[q]])
                for it in nl:
                    q = it["q"]
                    gcol = c.qkgt[:, 0:1] if it["co"] < 8 else c.qkgt[:, 1:2]
                    K.stt(q32[q][:], q32[q][:], gcol, rr[q][:], ALU.mult, ALU.mult, [bq32[q], brr[q], c.bconst], [bq32[q]])
                for it in items:
                    if it["nopos"]:
                        q = it["q"]
                        K.act(stn[q][:], q32[q][:], AF.Copy, [bq32[q]], [bstn[q]])
                        K.dma("sp", c.fmn[it["fi"] * 128:(it["fi"] + 1) * 128, cols], stn[q][:], [bstn[q]], [c.bfmn], part=True)
                rl = [it for it in items if it["rope"]]
                for it in rl:
                    q = it["q"]
                    it["p3"], it["bp3"] = PP.next()
                    K.mm(it["p3"][:], c.perm32[:], q32[q][:], True, True, [bq32[q], c.bconst], [it["bp3"]])
                for it in rl:
                    q = it["q"]
                    K.tt(t1[q][:], q32[q][:], rc[:, tok0:tok0 + 512], ALU.mult, [bq32[q], brc], [bt1[q]])
                for it in rl:
                    q = it["q"]
                    K.tt(t2[q][:], it["p3"][:], rs_[:, tok0:tok0 + 512], ALU.mult, [it["bp3"], brs], [bt2[q]])
                for it in rl:
                    q = it["q"]
                    K.tt(str_[q][:], t1[q][:], t2[q][:], ALU.add, [bt1[q], bt2[q]], [bstr[q]])
                    K.dma("sp", c.fmr[it["fi"] * 128:(it["fi"] + 1) * 128, cols], str_[q][:], [bstr[q]], [c.bfmr], part=True)
        S.barrier()


class AttnCtx:
    def __init__(self, c, ph, nS=3):
        self.c = c
        nc = c.nc
        self.nS = nS
        self.NP = 6
        self.P = [ph.enter_context(SBT(nc, "Pb%d" % i, [128, 512], BF16)) for i in range(6)]
        self.bP = [Buf() for _ in range(6)]
        self.pi = 0
        self.si = 0
        self.set = 0

    def block(self, N, kbs, nv, shape3=None):
        c = self.c
        K = c.K
        nS = self.nS
        if nv == 1:
            base = nS + 2 * (self.set % ((8 - nS) // 2))
            o = [(c.ps[base], c.bps[base])]
            sm = (c.ps[base + 1], c.bps[base + 1])
        else:
            base = nS
            o = [(c.ps[base + i], c.bps[base + i]) for i in range(nv)]
            sm = (c.ps[base + nv], c.bps[base + nv])
        aset = self.set % 2
        self.set += 1
        n = len(kbs)
        skew = nS - 1

        def view(ap):
            return ap

        def issue_s(i):
            kk, qq, vv, mask, reads, kp = kbs[i]
            sp_, bsp_ = c.ps[self.si % nS], c.bps[self.si % nS]
            self.si += 1
            K.mm(sp_[0:kp, 0:N], kk, qq, True, True, reads, [bsp_])
            pb = self.pi % self.NP
            self.pi += 1
            K.act(self.P[pb][0:kp, 0:N], sp_[0:kp, 0:N], AF.Exp, [bsp_], [self.bP[pb]], scale=SCALE)
            if mask is not None:
                K.tt(self.P[pb][0:kp, 0:N], self.P[pb][0:kp, 0:N], mask, ALU.mult, [self.bP[pb], c.bconst], [self.bP[pb]])
            return pb
        pend = [issue_s(i) for i in range(min(skew, n))]
        for i in range(n):
            if i + skew < n:
                pend.append(issue_s(i + skew))
            pb = pend[i]
            kk, qq, vv, mask, reads, kp = kbs[i]
            for vi in range(nv):
                K.mm(o[vi][0][:, 0:N], vv[vi], self.P[pb][0:kp, 0:N], i == 0, i == n - 1, [self.bP[pb]] + reads, [o[vi][1]])
            K.mm(sm[0][:, 0:N], c.onesb[0:kp, :], self.P[pb][0:kp, 0:N], i == 0, i == n - 1, [self.bP[pb], c.bc2], [sm[1]])
        return o, sm


def load_fm(c, q, dst, src_rows, src, col0, ncol, rbuf, wbuf, part=False):
    r0, r1 = src_rows
    c.K.dma(q, dst, src[r0 * 128:r1 * 128, col0:col0 + ncol].rearrange("(h p) t -> p h t", p=128), [rbuf], [wbuf], part=part)


def mod1_gen(c, ph):
    K = c.K
    l = 1
    wsl = [ph.enter_context(SBT(c.nc, "wb%d" % i, [128, 16, 512], BF16)) for i in range(2)]
    bwsl = [Buf(), Buf()]
    bm1 = Buf()

    def issue(s):
        K.dma("pool", wsl[s % 2][:], c.w_ada[l, :, s * 512:(s + 1) * 512].rearrange("(c p) n -> p c n", p=128), [], [bwsl[s % 2]])
    issue(0)
    issue(1)
    yield
    for s in range(24):
        w_ = s % 2
        p_, bp_ = c.ps[7], c.bps[7]
        for oc in range(4):
            for k in range(16):
                K.mm(p_[:, oc * 3:(oc + 1) * 3], wsl[w_][:, k, oc * 128:(oc + 1) * 128], c.cbf[:, k, :], k == 0, k == 15, [bwsl[w_], c.bc2], [bp_])
        for j in range(3):
            K.tt(c.modT[:, l, 4 * s:4 * s + 4, j], p_[:, j:12:3], c.badat[:, l, 4 * s:4 * s + 4], ALU.add, [bp_, c.bconst], [bm1], part=True)
        if s + 2 < 24:
            issue(s + 2)
        yield
    K.ts(c.modT[:, l, 16:32, :], c.modT[:, l, 16:32, :], 1.0, None, ALU.add, None, [bm1], [bm1])
    K.ts(c.modT[:, l, 64:80, :], c.modT[:, l, 64:80, :], 1.0, None, ALU.add, None, [bm1], [bm1])
    K.ts(c.modT[:, l, 32:48, :], c.modT[:, l, 32:48, :], 1.0 / ALPHA, None, ALU.mult, None, [bm1], [bm1])
    K.ts(c.modT[:, l, 80:96, :], c.modT[:, l, 80:96, :], 1.0 / ALPHA, None, ALU.mult, None, [bm1, c.bmod], [bm1, c.bmod])
    yield


def gqa_attn(c, A, ph, l, b, qrot, qnop, krot, kx, vA, bl, stg, bstg, rden, brden, sinkx, oc0, gen=None):
    K = c.K
    n_ = [0]
    for g in range(2):
        qlist = [("lat", qb) for qb in range(16)] + ([("ctx", qb) for qb in range(2)] if l == 0 else [])
        for kind, qb in qlist:
            kbs = []
            if kind == "lat":
                lk = [kb for kb in (qb - 1, qb, qb + 1) if 0 <= kb < 16] if l == 0 else list(range(16))
                for kb in lk:
                    mask = None
                    if l == 0 and kb == qb - 1:
                        mask = c.trimb[:, 0, :, :].rearrange("p h q -> p (h q)")
                    if l == 0 and kb == qb + 1:
                        mask = c.trimb[:, 1, :, :].rearrange("p h q -> p (h q)")
                    kbs.append((krot[:, g, kb * 128:(kb + 1) * 128], qrot[:, 4 * g:4 * g + 4, qb * 128:(qb + 1) * 128],
                                [vA[:, kb, g * 128:(g + 1) * 128]], mask, [bl], 128))
                qn = qnop[:, 4 * g:4 * g + 4, qb * 128:(qb + 1) * 128]
                tcol = b * 2048 + qb * 128
            else:
                qn = qnop[:, 4 * g:4 * g + 4, 2048 + qb * 128:2048 + (qb + 1) * 128]
                tcol = 4096 + b * 256 + qb * 128
            for cb in range(2):
                kbs.append((kx[:, g, cb * 128:(cb + 1) * 128], qn, [vA[:, 16 + cb, g * 128:(g + 1) * 128]], None, [bl], 128))
            o, sm = A.block(512, kbs, 1)
            if gen is not None:
                next(gen, None)
            i = n_[0] % 2
            n_[0] += 1
            if l == 0:
                K.tt(rden[i][:], sm[0][:], sinkx[:, g, :], ALU.add, [sm[1], c.bconst], [brden[i]])
                K.recip(rden[i][:], rden[i][:], [brden[i]], [brden[i]])
            else:
                K.recip(rden[i][:], sm[0][:], [sm[1]], [brden[i]])
            K.tt(stg[i][:], o[0][0][:], rden[i][:], ALU.mult, [o[0][1], brden[i]], [bstg[i]])
            K.dma("sp", c.oT[(oc0 + 4 * g) * 128:(oc0 + 4 * g + 4) * 128, tcol:tcol + 128].rearrange("(h p) t -> p h t", p=128),
                  stg[i][:].rearrange("p (h q) -> p h q", h=4), [bstg[i]], [c.boT], part=True)


def phase_attn(C, l):
    c = NS(C)
    nc, S, K, PP = c.nc, c.S, c.K, c.PP
    for b in range(2):
        with contextlib.ExitStack() as ph:
            def sb(n, s, d):
                return ph.enter_context(SBT(nc, n, s, d))
            A = AttnCtx(c, ph)
            qrot = sb("qrot", [128, 8, 2048], BF16); qnop = sb("qnop", [128, 8, 2304], BF16)
            krot = sb("krot", [128, 2, 2048], BF16); kx = sb("kx", [128, 2, 256], BF16)
            vA = sb("vA", [128, 18, 256], BF16)
            trimb = sb("trimb", [128, 2, 4, 128], BF16); trim32 = sb("trim32", [128, 2, 4, 128], F32)
            sinkx = sb("sinkx", [128, 2, 512], F32)
            stg = [sb("stg%d" % i, [128, 512], BF16) for i in range(2)]; bstg = [Buf(), Buf()]
            rden = [sb("rden%d" % i, [128, 512], F32) for i in range(2)]; brden = [Buf(), Buf()]
            bl = Buf(); bt_ = Buf()
            c.trimb = trimb
            K.dma("sp", trim32[:], c.trim, [], [bt_])
            K.cp(trimb[:], trim32[:], [bt_], [c.bconst])
            for h in range(8):
                K.ts(sinkx[:, h // 4, (h % 4) * 128:(h % 4 + 1) * 128], c.ones32[:], c.sinkt[:, h:h + 1], None, ALU.mult, None,
                     [c.bconst, c.bc2], [c.bconst])
            load_fm(c, "sp", qrot[:], (0, 8), c.fmr, b * 2048, 2048, c.bfmr, bl, part=True)
            load_fm(c, "sp", qnop[:, :, 0:2048], (0, 8), c.fmn, b * 2048, 2048, c.bfmn, bl, part=True)
            if l == 0:
                load_fm(c, "sp", qnop[:, :, 2048:2304], (0, 8), c.fmn, 4096 + b * 256, 256, c.bfmn, bl, part=True)
            load_fm(c, "sp", krot[:], (8, 10), c.fmr, b * 2048, 2048, c.bfmr, bl, part=True)
            load_fm(c, "sp", kx[:], (8, 10), c.fmn, 4096 + b * 256, 256, c.bfmn, bl, part=True)
            K.dma("sp", vA[:, 0:16, :], c.vtok[b * 2048:(b + 1) * 2048, 0:256].rearrange("(k p) n -> p k n", p=128), [c.bvtok], [bl], part=True)
            K.dma("sp", vA[:, 16:18, :], c.vtok[4096 + b * 256:4096 + (b + 1) * 256, 0:256].rearrange("(k p) n -> p k n", p=128), [c.bvtok], [bl], part=True)
            gen = mod1_gen(c, ph) if (l == 0 and b == 0) else None
            gqa_attn(c, A, ph, l, b, qrot, qnop, krot, kx, vA, bl, stg, bstg, rden, brden, sinkx, 0, gen)
            if gen is not None:
                for _ in gen:
                    pass
            S.barrier()
        if l == 0:
            attn_B(c, b)
        else:
            attn_D(c, b)


def attn_B(c, b):
    nc, S, K = c.nc, c.S, c.K
    with contextlib.ExitStack() as ph:
        def sb(n, s, d):
            return ph.enter_context(SBT(nc, n, s, d))
        A = AttnCtx(c, ph, 2)
        qB = sb("qB", [128, 8, 2304], BF16); kB = sb("kB", [128, 8, 2304], BF16)
        vB64 = sb("vB64", [64, 32, 1024], BF16); vBx = sb("vBx", [128, 2, 1024], BF16)
        Tt = sb("Tt", [64, 8, 15, 64], BF16)
        rp32 = [sb("rp32_%d" % i, [64, 15, 64], F32) for i in range(2)]; brp = [Buf(), Buf()]
        cm = sb("cm", [64, 64], F32); bcm = Buf(); bT = Buf(); bl = Buf()
        Pl = [sb("Pl%d" % i, [64, 512], BF16) for i in range(2)]; bPl = [Buf(), Buf()]
        Pc = [sb("Pc%d" % i, [128, 128], BF16) for i in range(2)]; bPc = [Buf(), Buf()]
        stg = [sb("stgB%d" % i, [128, 512], BF16) for i in range(2)]; bstg = [Buf(), Buf()]
        rden = [sb("rdenB%d" % i, [128, 512], F32) for i in range(2)]; brden = [Buf(), Buf()]
        K.dma("sp", cm[:], c.cmask, [], [bcm])
        for h in range(8):
            i = h % 2
            K.dma("sp", rp32[i][:], c.rpbx[:, h, :, :], [], [brp[i]])
            K.act(rp32[i][:], rp32[i][:], AF.Exp, [brp[i]], [brp[i]])
            K.tt(Tt[:, h, :, :], rp32[i][:], cm[:].unsqueeze(1).to_broadcast([64, 15, 64]), ALU.mult, [brp[i], bcm], [bT], part=True)
        load_fm(c, "sp", qB[:, :, 0:2048], (10, 18), c.fmn, b * 2048, 2048, c.bfmn, bl, part=True)
        load_fm(c, "sp", qB[:, :, 2048:2304], (10, 18), c.fmn, 4096 + b * 256, 256, c.bfmn, bl, part=True)
        load_fm(c, "sp", kB[:, :, 0:2048], (18, 26), c.fmn, b * 2048, 2048, c.bfmn, bl, part=True)
        load_fm(c, "sp", kB[:, :, 2048:2304], (18, 26), c.fmn, 4096 + b * 256, 256, c.bfmn, bl, part=True)
        for hh in range(2):
            K.dma("sp", vB64[:, hh * 16:(hh + 1) * 16, :],
                  c.vtok[b * 2048 + hh * 1024:b * 2048 + (hh + 1) * 1024, 256:1280].rearrange("(r p) n -> p r n", p=64), [c.bvtok], [bl], part=True)
        K.dma("sp", vBx[:], c.vtok[4096 + b * 256:4096 + (b + 1) * 256, 256:1280].rearrange("(k p) n -> p k n", p=128), [c.bvtok], [bl], part=True)
        items = [(h, rg, r) for h in range(8) for rg in range(4) for r in range(rg * 8, rg * 8 + 8)]

        def stage_s(n_):
            h, rg, r = items[n_]
            r0 = min(max(r - 4, 0), 24)
            d0 = r0 - r + 7
            i = n_ % 2
            sl, bsl = c.ps[i], c.bps[i]
            sc_, bsc = c.ps[2 + i], c.bps[2 + i]
            qq = qB[:, h, r * 64:(r + 1) * 64]
            for kr in range(8):
                K.mm(sl[0:64, kr * 64:(kr + 1) * 64], kB[:, h, (r0 + kr) * 64:(r0 + kr + 1) * 64], qq, True, True, [bl], [bsl], part=True)
            for cb in range(2):
                K.mm(sc_[:, cb * 64:(cb + 1) * 64], kB[:, h, 2048 + cb * 128:2048 + (cb + 1) * 128], qq, True, True, [bl], [bsc], part=True)
            K.act(Pl[i][:], sl[0:64, :], AF.Exp, [bsl], [bPl[i]], scale=SCALE)
            K.act(Pc[i][:], sc_[:, 0:128], AF.Exp, [bsc], [bPc[i]], scale=SCALE)
            K.tt(Pl[i][:].rearrange("p (a q) -> p a q", a=8), Pl[i][:].rearrange("p (a q) -> p a q", a=8), Tt[:, h, d0:d0 + 8, :], ALU.mult,
                 [bPl[i], bT], [bPl[i]])

        def stage_pv(n_):
            h, rg, r = items[n_]
            r0 = min(max(r - 4, 0), 24)
            i = n_ % 2
            st = (h * 4 + rg) % 2
            o_ps, bo = c.ps[4 + 2 * st], c.bps[4 + 2 * st]
            s_ps, bs = c.ps[5 + 2 * st], c.bps[5 + 2 * st]
            oc = slice((r % 8) * 64, (r % 8 + 1) * 64)
            for kr in range(8):
                K.mm(o_ps[:, oc], vB64[:, r0 + kr, h * 128:(h + 1) * 128], Pl[i][:, kr * 64:(kr + 1) * 64], kr == 0, False, [bPl[i], bl], [bo], part=True)
            for cb in range(2):
                K.mm(o_ps[:, oc], vBx[:, cb, h * 128:(h + 1) * 128], Pc[i][:, cb * 64:(cb + 1) * 64], False, cb == 1, [bPc[i], bl], [bo], part=True)
            for kr in range(8):
                K.mm(s_ps[:, oc], c.onesb[0:64, :], Pl[i][:, kr * 64:(kr + 1) * 64], kr == 0, False, [bPl[i], c.bc2], [bs], part=True)
            for cb in range(2):
                K.mm(s_ps[:, oc], c.onesb[:, :], Pc[i][:, cb * 64:(cb + 1) * 64], False, cb == 1, [bPc[i], c.bc2], [bs], part=True)
            if r % 8 == 7:
                K.recip(rden[st][:], s_ps[:], [bs], [brden[st]])
                K.tt(stg[st][:], o_ps[:], rden[st][:], ALU.mult, [bo, brden[st]], [bstg[st]])
                tcol = b * 2048 + rg * 512
                K.dma("sp", c.oT[(8 + h) * 128:(9 + h) * 128, tcol:tcol + 512], stg[st][:], [bstg[st]], [c.boT], part=True)
        stage_s(0)
        for n_ in range(len(items)):
            if n_ + 1 < len(items):
                stage_s(n_ + 1)
            stage_pv(n_)
        for h in range(8):
            kbs = []
            for cb in range(2):
                kbs.append((kB[:, h, 2048 + cb * 128:2048 + (cb + 1) * 128], qB[:, h, 2048:2304], [vBx[:, cb, h * 128:(h + 1) * 128]], None, [bl], 128))
            o, sm = A.block(256, kbs, 1)
            st = h % 2
            K.recip(rden[st][:, 0:256], sm[0][:, 0:256], [sm[1]], [brden[st]])
            K.tt(stg[st][:, 0:256], o[0][0][:, 0:256], rden[st][:, 0:256], ALU.mult, [o[0][1], brden[st]], [bstg[st]])
            tcol = 4096 + b * 256
            K.dma("sp", c.oT[(8 + h) * 128:(9 + h) * 128, tcol:tcol + 256], stg[st][:, 0:256], [bstg[st]], [c.boT], part=True)
        S.barrier()


def attn_D(c, b):
    nc, S, K = c.nc, c.S, c.K
    with contextlib.ExitStack() as ph:
        def sb(n, s, d):
            return ph.enter_context(SBT(nc, n, s, d))
        A = AttnCtx(c, ph, 4)
        dq = sb("dq", [128, 8, 2048], BF16); dqn = sb("dqn", [128, 8, 2048], BF16)
        dk = sb("dk", [128, 8, 2048], BF16); dkx = sb("dkx", [128, 8, 256], BF16)
        vD = sb("vD", [128, 18, 1024], BF16)
        o0 = sb("o0", [128, 2, 512], F32); od = sb("od", [128, 2, 512], F32); sqd = sb("sqd", [128, 2, 512], F32)
        r0_ = sb("r0_", [128, 512], F32); r1_ = sb("r1_", [128, 512], F32); rs2 = sb("rs2", [128, 512], F32)
        stg = [sb("stgD%d" % i, [128, 512], BF16) for i in range(2)]; bstg = [Buf(), Buf()]
        bl = Buf(); bo0 = Buf(); bod = Buf(); bsq = Buf(); br0 = Buf(); br1 = Buf(); brs2 = Buf()
        load_fm(c, "sp", dq[:], (10, 18), c.fmr, b * 2048, 2048, c.bfmr, bl, part=True)
        load_fm(c, "sp", dqn[:], (10, 18), c.fmn, b * 2048, 2048, c.bfmn, bl, part=True)
        load_fm(c, "sp", dk[:], (18, 26), c.fmr, b * 2048, 2048, c.bfmr, bl, part=True)
        load_fm(c, "sp", dkx[:], (18, 26), c.fmn, 4096 + b * 256, 256, c.bfmn, bl, part=True)
        for hh in range(2):
            K.dma("sp", vD[:, hh * 8:(hh + 1) * 8, :],
                  c.vtok[b * 2048 + hh * 1024:b * 2048 + (hh + 1) * 1024, 256:1280].rearrange("(k p) n -> p k n", p=128), [c.bvtok], [bl], part=True)
        K.dma("sp", vD[:, 16:18, :], c.vtok[4096 + b * 256:4096 + (b + 1) * 256, 256:1280].rearrange("(k p) n -> p k n", p=128), [c.bvtok], [bl], part=True)
        n_ = 0
        for hd in range(4):
            for qt in range(4):
                qs = slice(qt * 512, (qt + 1) * 512)
                for t in range(2):
                    cc = 2 * hd + t
                    kbs = []
                    for kb in range(16):
                        kbs.append((dk[:, cc, kb * 128:(kb + 1) * 128], dq[:, cc, qs],
                                    [vD[:, kb, hd * 256:hd * 256 + 128], vD[:, kb, hd * 256 + 128:hd * 256 + 256]], None, [bl], 128))
                    for cb in range(2):
                        kbs.append((dkx[:, cc, cb * 128:(cb + 1) * 128], dqn[:, cc, qs],
                                    [vD[:, 16 + cb, hd * 256:hd * 256 + 128], vD[:, 16 + cb, hd * 256 + 128:hd * 256 + 256]], None, [bl], 128))
                    o, sm = A.block(512, kbs, 2)
                    if t == 0:
                        K.recip(r0_[:], sm[0][:], [sm[1]], [br0])
                        for vi in range(2):
                            K.tt(o0[:, vi, :], o[vi][0][:], r0_[:], ALU.mult, [o[vi][1], br0], [bo0], part=(vi == 1))
                    else:
                        K.recip(r1_[:], sm[0][:], [sm[1]], [br1])
                        K.ts(r1_[:], r1_[:], c.lamt[:, 6:7], None, ALU.mult, None, [br1, c.bconst], [br1])
                        for vi in range(2):
                            K.tt(od[:, vi, :], o[vi][0][:], r1_[:], ALU.mult, [o[vi][1], br1], [bod], part=(vi == 1))
                        K.tt(od[:], od[:], o0[:], ALU.add, [bod, bo0], [bod])
                K.act(sqd[:], od[:], AF.Square, [bod], [bsq])
                p_, bp_ = c.ps[7], c.bps[7]
                K.mm(p_[:], c.ones32[:], sqd[:, 0, :], True, False, [bsq, c.bc2], [bp_])
                K.mm(p_[:], c.ones32[:], sqd[:, 1, :], False, True, [bsq, c.bc2], [bp_])
                K.rsqrt(rs2[:], p_[:], 256.0 * 1e-6, [bp_], [brs2])
                for vi in range(2):
                    i = n_ % 2
                    n_ += 1
                    K.stt(stg[i][:], od[:, vi, :], c.sublnt[:, vi:vi + 1], rs2[:], ALU.mult, ALU.mult, [bod, brs2, c.bconst], [bstg[i]])
                    tcol = b * 2048 + qt * 512
                    K.dma("sp", c.oT[(8 + 2 * hd + vi) * 128:(9 + 2 * hd + vi) * 128, tcol:tcol + 512], stg[i][:], [bstg[i]], [c.boT], part=True)
        S.barrier()


def ln_tile(c, K, x32, bxk, lcol, gcol_fn, eps, zsq, bzsq, mean, msq, var, rstd, bst, post):
    p_s, bp_s = c.ps[6], c.bps[6]
    p_q, bp_q = c.ps[7], c.bps[7]
    for k in range(16):
        i = k % 2
        K.act(zsq[i][:], x32[:, k, :], AF.Square, [bxk[k]], [bzsq[i]])
        K.mm(p_s[:], c.ones32[:], x32[:, k, :], k == 0, k == 15, [bxk[k], c.bc2], [bp_s])
        K.mm(p_q[:], c.ones32[:], zsq[i][:], k == 0, k == 15, [bzsq[i], c.bc2], [bp_q])
    K.ts(mean[:], p_s[:], 1.0 / D, None, ALU.mult, None, [bp_s], [bst])
    K.tt(msq[:], mean[:], mean[:], ALU.mult, [bst], [bst])
    K.stt(var[:], p_q[:], 1.0 / D, msq[:], ALU.mult, ALU.subtract, [bp_q, bst], [bst])
    K.rsqrt(rstd[:], var[:], eps, [bst], [bst])
    for k in range(16):
        K.tt(x32[:, k, :], x32[:, k, :], mean[:], ALU.subtract, [bxk[k], bst], [bxk[k]])
        K.tt(x32[:, k, :], x32[:, k, :], rstd[:], ALU.mult, [bxk[k], bst], [bxk[k]])
        post(k)


def phase_oproj(C, l):
    c = NS(C)
    nc, S, K, PP = c.nc, c.S, c.K, c.PP
    src = c.xin if l == 0 else c.xs
    ntile = 9 if l == 0 else 8
    with contextlib.ExitStack() as ph:
        def sb(n, s, d):
            return ph.enter_context(SBT(nc, n, s, d))
        wo = sb("wo", [128, 16, 2048], BF16); bwo = Buf()
        for i in range(4):
            K.dma("pool", wo[:, 4 * i:4 * i + 4, :], c.w_out[l][512 * i:512 * (i + 1), :].rearrange("(c p) n -> p c n", p=128), [], [bwo], part=True)
        x32 = sb("x32o", [128, 16, 512], F32); bxk = [Buf() for _ in range(16)]
        h232 = sb("h232", [128, 16, 512], F32); bh2 = Buf()
        ot = [sb("ot%d" % i, [128, 16, 512], BF16) for i in range(2)]; bot = [Buf(), Buf()]
        zsq = [sb("zsq%d" % i, [128, 512], F32) for i in range(2)]; bzsq = [Buf(), Buf()]
        mean = sb("mean", [128, 512], F32); msq = sb("msq", [128, 512], F32); var = sb("var", [128, 512], F32); rstd = sb("rstd", [128, 512], F32)
        bst = Buf()
        sc = sb("rsc", [128, 64], F32); bi = sb("rbi", [128, 64], F32); tmp = sb("rtmp", [128, 64], F32); mk = sb("rmk", [128, 64], F32)
        m1 = sb("rm1", [128, 16], F32); m2 = sb("rm2", [128, 16], F32); gs = sb("rgs", [128, 16], F32); pen = sb("rpen", [128, 16], F32)
        gm = sb("rgm", [128, 4], F32); t1 = sb("rt1", [128, 4], F32); t2 = sb("rt2", [128, 4], F32); wsum = sb("rws", [128, 4], F32)
        gates = sb("gates", [128, 64], F32)
        h2tm = [sb("h2tm%d" % i, [128, 2048], BF16) for i in range(2)]; bh2tm = [Buf(), Buf()]
        br = Buf(); bgt = Buf()
        for tt in range(ntile):
            lat = tt < 8
            j = tt // 4 if lat else 2
            cols = slice(tt * 512, (tt + 1) * 512)
            for hk in range(2):
                K.dma("sp", x32[:, hk * 8:(hk + 1) * 8, :], src[hk * 1024:(hk + 1) * 1024, cols].rearrange("(c p) t -> p c t", p=128), [c.bxs[tt]], bxk[hk * 8:(hk + 1) * 8])
            o_ = tt % 2
            K.dma("sp", ot[o_][:], c.oT[:, cols].rearrange("(c p) t -> p c t", p=128), [c.boT], [bot[o_]])
            for co in range(16):
                p_, bp_ = PP.next(0, 6)
                for k in range(16):
                    K.mm(p_[:], wo[:, k, co * 128:(co + 1) * 128], ot[o_][:, k, :], k == 0, k == 15, [bwo, bot[o_]], [bp_])
                K.stt(x32[:, co, :], p_[:], c.modT[:, l, 32 + co, j:j + 1], x32[:, co, :], ALU.mult, ALU.add, [bp_, c.bmod, bxk[co]], [bxk[co]])

            def post(k):
                K.act(x32[:, k, :], x32[:, k, :], AF.Identity, [bxk[k], c.bconst], [bxk[k]], bias=c.lnt[:, l, 1, k:k + 1], scale=c.lnt[:, l, 0, k:k + 1])
                K.act(h232[:, k, :], x32[:, k, :], AF.Identity, [bxk[k], c.bmod], [bh2], bias=c.modT[:, l, 48 + k, j:j + 1], scale=c.modT[:, l, 64 + k, j:j + 1], part=True)
                K.cp(ot[o_][:, k, :], h232[:, k, :], [bh2], [bot[o_]], part=True)
            ln_tile(c, K, x32, bxk, None, None, LN_EPS, zsq, bzsq, mean, msq, var, rstd, bst, post)
            K.dma("sp", c.xs[:, cols].rearrange("(c p) t -> p c t", p=128), x32[:], bxk, [c.bxs[tt]])
            for tb in range(4):
                m_ = tb % 2
                for hf in range(2):
                    p_, bp_ = PP.next(0, 6)
                    pv = p_[:].bitcast(BF16)
                    for kk in range(8):
                        k = hf * 8 + kk
                        K.tr(pv[:, kk * 128:(kk + 1) * 128], ot[o_][:, k, tb * 128:(tb + 1) * 128], c.idb[:], [bot[o_], c.bc2], [bp_], part=True)
                    K.cp(h2tm[m_][:, hf * 1024:(hf + 1) * 1024], pv[:, 0:1024], [bp_], [bh2tm[m_]], part=(hf == 1))
                r0 = tt * 512 + tb * 128
                K.dma("sp", c.h2tok[r0:r0 + 128, :], h2tm[m_][:], [bh2tm[m_]], [c.bh2tok], part=True)
            for tb in range(4):
                p_, bp_ = PP.next(0, 6)
                for k in range(16):
                    K.mm(p_[:, 0:16], h232[:, k, tb * 128:(tb + 1) * 128], c.wrt[:, k, :], k == 0, k == 15, [bh2, c.bconst], [bp_])
                K.act(sc[:, tb * 16:(tb + 1) * 16], p_[:, 0:16], AF.Sigmoid, [bp_], [br], part=True)
            v3 = lambda t: t[:].rearrange("p (a e) -> p a e", e=4)
            v16 = lambda t: t[:].rearrange("p (a e) -> p a e", e=16)
            R = [br, c.bconst]
            K.tt(bi[:], sc[:], c.rbt[:], ALU.add, R, [br])
            K.red(m1[:], v3(bi), ALU.max, R, [br])
            K.tt(v3(tmp), v3(bi), m1[:].unsqueeze(2).to_broadcast([128, 16, 4]), ALU.is_equal, R, [br])
            K.stt(tmp[:], tmp[:], -BIG, bi[:], ALU.mult, ALU.add, R, [br])
            K.red(m2[:], v3(tmp), ALU.max, R, [br])
            K.tt(gs[:], m1[:], m2[:], ALU.add, R, [br])
            K.red(gm[:], gs[:].rearrange("p (a g) -> p a g", g=4), ALU.max, R, [br])
            K.tt(pen[:].rearrange("p (a g) -> p a g", g=4), gs[:].rearrange("p (a g) -> p a g", g=4),
                 gm[:].unsqueeze(2).to_broadcast([128, 4, 4]), ALU.is_ge, R, [br])
            K.ts(pen[:], pen[:], BIG, -BIG, ALU.mult, ALU.add, R, [br])
            K.tt(v3(mk), v3(bi), pen[:].unsqueeze(2).to_broadcast([128, 16, 4]), ALU.add, R, [br])
            K.red(t1[:], v16(mk), ALU.max, R, [br])
            K.tt(v16(tmp), v16(mk), t1[:].unsqueeze(2).to_broadcast([128, 4, 16]), ALU.is_equal, R, [br])
            K.stt(tmp[:], tmp[:], -BIG, mk[:], ALU.mult, ALU.add, R, [br])
            K.red(t2[:], v16(tmp), ALU.max, R, [br])
            K.tt(v16(tmp), v16(mk), t2[:].unsqueeze(2).to_broadcast([128, 4, 16]), ALU.is_ge, R, [br])
            K.cp(c.OH[:, tt * 4:(tt + 1) * 4, :], v16(tmp), R, [c.bOH], part=True)
            K.tt(tmp[:], tmp[:], sc[:], ALU.mult, R, [br])
            K.red(wsum[:], v16(tmp), ALU.add, R, [br])
            K.recip(wsum[:], wsum[:], R, [br])
            K.tt(v16(gates), v16(tmp), wsum[:].unsqueeze(2).to_broadcast([128, 4, 16]), ALU.mult, R, [br])
            K.cp(c.GS[:, tt * 4:(tt + 1) * 4, :], v16(gates), R, [c.bGS], part=True)
        S.barrier()


def phase_route(C, l):
    c = NS(C)
    nc, S, K, PP = c.nc, c.S, c.K, c.PP
    nblk = 36 if l == 0 else 32
    NU = 33 if l == 0 else 31
    with contextlib.ExitStack() as ph:
        def sb(n, s, d):
            return ph.enter_context(SBT(nc, n, s, d))
        R = sb("R", [128, 36, 16], F32); Bc = sb("Bc", [128, 36, 16], F32); run = sb("run", [128, 37, 16], F32)
        val = sb("val", [128, 36, 16], F32); valm = sb("valm", [128, 36, 16], F32); isA = sb("isA", [128, 36, 16], F32)
        pA = sb("pA", [128, 36], F32); pB = sb("pB", [128, 36], F32); gA = sb("gA", [128, 36], F32); gT_ = sb("gTt", [128, 36], F32)
        nun = sb("nun", [128, 16], F32); off = sb("off", [128, 16], F32); cmp3 = sb("cmp3", [128, 16], F32); esel = sb("esel", [128, 1], F32)
        ugf = sb("ugf", [128, 33], F32); ugs = sb("ugs", [128, 33], F32); ugd = sb("ugd", [128, 33], F32)
        hb_ = [sb("hbr%d" % i, [128, 2048], BF16) for i in range(2)]; bhb = [Buf(), Buf()]
        b = Buf()
        OHs = c.OH[:, 0:nblk, :]
        for hf in range(2):
            b0, b1 = hf * (nblk // 2), (hf + 1) * (nblk // 2)
            n16 = (b1 - b0) * 16
            OHf = c.OH[:, b0:b1, :].rearrange("p a g -> p (a g)")
            p1, bp1 = c.ps[2 * hf], c.bps[2 * hf]
            K.mm(p1[:, 0:n16], c.ltri32[:], OHf, True, True, [c.bOH, c.bconst], [bp1])
            K.cp(R[:, b0:b1, :].rearrange("p a g -> p (a g)"), p1[:, 0:n16], [bp1], [b])
            p2, bp2 = c.ps[2 * hf + 1], c.bps[2 * hf + 1]
            K.mm(p2[:, 0:n16], c.ones32[:], OHf, True, True, [c.bOH, c.bc2], [bp2])
            K.cp(Bc[:, b0:b1, :].rearrange("p a g -> p (a g)"), p2[:, 0:n16], [bp2], [b])
        K.memset(run[:, 0, :], 0.0, [b])
        for blk in range(nblk):
            K.tt(run[:, blk + 1, :], run[:, blk, :], Bc[:, blk, :], ALU.add, [b], [b])
        K.memset(nun[:], 0.0, [b])
        for m in range(9):
            K.stt(nun[:], run[:, nblk, :], float(512 * m), nun[:], ALU.is_gt, ALU.add, [b], [b])
        K.memset(off[:], 0.0, [b])
        for e in range(1, 16):
            K.stt(off[:, e:e + 1], nun[:, e - 1:e], 512.0, off[:, e - 1:e], ALU.mult, ALU.add, [b], [b])
        K.tt(val[:, 0:nblk, :], R[:, 0:nblk, :], run[:, 0:nblk, :], ALU.add, [b], [b])
        K.tt(val[:, 0:nblk, :], val[:, 0:nblk, :], off[:].unsqueeze(1).to_broadcast([128, nblk, 16]), ALU.add, [b], [b])
        K.tt(valm[:, 0:nblk, :], val[:, 0:nblk, :], OHs, ALU.mult, [b, c.bOH], [b])
        K.red(pB[:, 0:nblk], valm[:, 0:nblk, :], ALU.max, [b], [b])
        K.stt(valm[:, 0:nblk, :], OHs, -1.0e6, val[:, 0:nblk, :], ALU.mult, ALU.add, [b, c.bOH], [b])
        K.ts(valm[:, 0:nblk, :], valm[:, 0:nblk, :], 1.0e6, None, ALU.add, None, [b], [b])
        K.red(pA[:, 0:nblk], valm[:, 0:nblk, :], ALU.min, [b], [b])
        K.tt(isA[:, 0:nblk, :], valm[:, 0:nblk, :], pA[:, 0:nblk].unsqueeze(2).to_broadcast([128, nblk, 16]), ALU.is_equal, [b], [b])
        K.tt(isA[:, 0:nblk, :], isA[:, 0:nblk, :], c.GS[:, 0:nblk, :], ALU.mult, [b, c.bGS], [b])
        K.red(gA[:, 0:nblk], isA[:, 0:nblk, :], ALU.add, [b], [b])
        K.red(gT_[:, 0:nblk], c.GS[:, 0:nblk, :], ALU.add, [b, c.bGS], [b])
        K.memset(c.GSA[:], 0.0, [c.bPOS]); K.memset(c.GSB[:], 0.0, [c.bPOS])
        K.cp(c.GSA[:, 0:nblk, 0], gA[:, 0:nblk], [b], [c.bPOS], part=True)
        K.tt(c.GSB[:, 0:nblk, 0], gT_[:, 0:nblk], gA[:, 0:nblk], ALU.subtract, [b], [c.bPOS], part=True)
        K.ts(pA[:, 0:nblk], pA[:, 0:nblk], float(NU * 512 - 1), 0.0, ALU.min, ALU.max, [b], [b])
        K.ts(pB[:, 0:nblk], pB[:, 0:nblk], float(NU * 512 - 1), 0.0, ALU.min, ALU.max, [b], [b])
        K.cp(c.POSA[:, 0:nblk], pA[:, 0:nblk], [b], [c.bPOS], part=True)
        K.cp(c.POSB[:, 0:nblk], pB[:, 0:nblk], [b], [c.bPOS], part=True)
        for u in range(NU):
            K.ts(cmp3[:, 0:15], off[:, 1:16], float(512 * u), None, ALU.is_le, None, [b], [b])
            K.red(esel[:], cmp3[:, 0:15], ALU.add, [b], [b])
            K.ts(ugf[:, u:u + 1], esel[:], float(16 * l), float(16 * l + 15), ALU.add, ALU.min, [b], [b])
        K.ts(ugs[:, 0:NU], ugf[:, 0:NU], 4096.0, None, ALU.mult, None, [b], [b])
        K.ts(ugd[:, 0:NU], ugf[:, 0:NU], 1024.0, None, ALU.mult, None, [b], [b])
        for u in range(NU):
            K.ts(c.IDXG[:, u, :], c.tgt[:, 0:32], ugs[:, u:u + 1], None, ALU.add, None, [b, c.bconst], [c.bIDX], part=True)
            K.ts(c.IDXD[:, u, :], c.tgt[:, 32:40], ugd[:, u:u + 1], None, ALU.add, None, [b, c.bconst], [c.bIDX], part=True)
        if c.dbg and l == 0:
            K.dma("sp", c.dposa, c.POSA[:], [c.bPOS], [Buf()]); K.dma("sp", c.dposb, c.POSB[:], [c.bPOS], [Buf()])
            K.dma("sp", c.didxg, c.IDXG[:].rearrange("p a b -> p (a b)"), [c.bIDX], [Buf()])
            K.dma("sp", c.didxd, c.IDXD[:].rearrange("p a b -> p (a b)"), [c.bIDX], [Buf()])
            K.dma("sp", c.dgsa, c.GSA[:].rearrange("p a b -> p (a b)"), [c.bPOS], [Buf()])
            K.dma("sp", c.doh, c.OH[:].rearrange("p a b -> p (a b)"), [c.bOH], [Buf()])
        for blk in range(nblk):
            i = blk % 2
            K.dma("sp", hb_[i][:], c.h2tok[blk * 128:(blk + 1) * 128, :], [c.bh2tok], [bhb[i]])
            for (POS_, GS_) in ((c.POSA, c.GSA), (c.POSB, c.GSB)):
                idx = POS_[:, blk:blk + 1]
                S.op("pool", lambda e, i=i, idx=idx: e.indirect_dma_start(out=c.h2sorted[:, :], out_offset=bass.IndirectOffsetOnAxis(ap=idx, axis=0),
                                                                          in_=hb_[i][:, :], in_offset=None),
                     [bhb[i], c.bPOS, c.bzero], [c.bh2s], dma=True, part=True)
                gs_ap = GS_[:, blk, :]
                S.op("pool", lambda e, gs_ap=gs_ap, idx=idx: e.indirect_dma_start(out=c.gsorted[:, :], out_offset=bass.IndirectOffsetOnAxis(ap=idx, axis=0),
                                                                                  in_=gs_ap, in_offset=None),
                     [c.bPOS, c.bzero], [c.bgs], dma=True, part=True)
        S.barrier()


def phase_moe(C, l):
    c = NS(C)
    nc, S, K, PP = c.nc, c.S, c.K, c.PP
    NU = 33 if l == 0 else 31
    with contextlib.ExitStack() as ph:
        def sb(n, s, d):
            return ph.enter_context(SBT(nc, n, s, d))
        hs = sb("hs0", [128, 4, 2048], BF16); bhs = Buf()
        gsl = [sb("gsl%d" % i, [128, 4, 4], F32) for i in range(2)]; bgsl = [Buf(), Buf()]
        hT = sb("hT", [128, 16, 512], BF16); bhT = Buf()
        ae = sb("ae", [128, 8, 512], BF16); bae = Buf()
        acc = sb("acc", [128, 4, 2048], BF16); bacc = [Buf() for _ in range(4)]
        wgs = [sb("wgs%d" % i, [128, 16, 512], BF16) for i in range(2)]; bwg = [Buf(), Buf()]
        wus = [sb("wus%d" % i, [128, 16, 512], BF16) for i in range(2)]; bwu = [Buf(), Buf()]
        wds1 = sb("wds1", [128, 8, 2048], BF16); bwd1 = Buf()
        sil = [sb("sil%d" % i, [128, 512], F32) for i in range(2)]; bsil = [Buf(), Buf()]
        wgv = c.wg.rearrange("e k (h n) -> (e k h) n", h=2)
        wuv = c.wu.rearrange("e k (h n) -> (e k h) n", h=2)
        wdv = c.wd.rearrange("e f n -> (e f) n")
        st_ = dict(wi=0, si=0)

        def gat(dst, src2d, idx, wbuf):
            nrow = src2d.shape[0]
            S.op("pool", lambda e: e.indirect_dma_start(out=dst, out_offset=None, in_=src2d, in_offset=bass.IndirectOffsetOnAxis(ap=idx, axis=0)),
                 [c.bIDX], [wbuf], dma=True, part=True)

        def load(u):
            rows = slice(u * 512, (u + 1) * 512)
            K.dma("sp", hs[:], c.h2sorted[rows, :].rearrange("(a p) d -> p a d", p=128), [c.bh2s], [bhs])
            K.dma("sp", gsl[u % 2][:], c.gsorted[rows, :].rearrange("(a p) e -> p a e", p=128), [c.bgs], [bgsl[u % 2]])

        def transposes(u):
            for sbk in range(4):
                for hf in range(2):
                    p_, bp_ = PP.next(0, 4)
                    pv = p_[:].bitcast(BF16)
                    for kk in range(8):
                        k = hf * 8 + kk
                        K.tr(pv[:, kk * 128:(kk + 1) * 128], hs[:, sbk, k * 128:(k + 1) * 128], c.idb[:], [bhs, c.bc2], [bp_], part=True)
                    K.cp(hT[:, hf * 8:(hf + 1) * 8, sbk * 128:(sbk + 1) * 128], pv[:, 0:1024].rearrange("p (k t) -> p k t", k=8), [bp_], [bhT], part=True)

        def gateup(u):
            for hf in range(2):
                w_ = st_["wi"] % 2; st_["wi"] += 1
                for k in range(16):
                    gat(wgs[w_][:, k, :], wgv[:, :], c.IDXG[:, u, 2 * k + hf:2 * k + hf + 1], bwg[w_])
                for k in range(16):
                    gat(wus[w_][:, k, :], wuv[:, :], c.IDXG[:, u, 2 * k + hf:2 * k + hf + 1], bwu[w_])
                for jj in range(4):
                    j = hf * 4 + jj
                    pg, bpg = PP.next(0, 4)
                    for k in range(16):
                        K.mm(pg[:], wgs[w_][:, k, jj * 128:(jj + 1) * 128], hT[:, k, :], k == 0, k == 15, [bwg[w_], bhT], [bpg])
                    pu, bpu = PP.next(0, 4)
                    for k in range(16):
                        K.mm(pu[:], wus[w_][:, k, jj * 128:(jj + 1) * 128], hT[:, k, :], k == 0, k == 15, [bwu[w_], bhT], [bpu])
                    s_ = st_["si"] % 2; st_["si"] += 1
                    K.act(sil[s_][:], pg[:], AF.Silu, [bpg], [bsil[s_]])
                    K.tt(ae[:, j, :], sil[s_][:], pu[:], ALU.mult, [bsil[s_], bpu], [bae], part=True)
            for j in range(8):
                gat(wds1[:, j, :], wdv[:, :], c.IDXD[:, u, j:j + 1], bwd1)

        def down(u):
            i = u % 2
            for sbk in range(4):
                for dg in range(4):
                    po, bpo = PP.next(4, 8)
                    dc = slice(dg * 512, (dg + 1) * 512)
                    for j in range(8):
                        K.mm(po[:], ae[:, j, sbk * 128:(sbk + 1) * 128], wds1[:, j, dc], j == 0, j == 7, [bwd1, bae], [bpo])
                    K.ts(acc[:, sbk, dc], po[:], gsl[i][:, sbk, 0:1], None, ALU.mult, None, [bpo, bgsl[i]], [bacc[sbk]], part=True)
                r0 = u * 512 + sbk * 128
                K.dma("sp", c.fsorted[r0:r0 + 128, :], acc[:, sbk, :], [bacc[sbk]], [c.bfs], part=True)
        load(0)
        transposes(0)
        for u in range(NU):
            if u + 1 < NU:
                load(u + 1)
            gateup(u)
            if u + 1 < NU:
                transposes(u + 1)
            down(u)
        S.barrier()


def phase_ln2(C, l):
    c = NS(C)
    nc, S, K, PP = c.nc, c.S, c.K, c.PP
    ntile = 9 if l == 0 else 8
    with contextlib.ExitStack() as ph:
        def sb(n, s, d):
            return ph.enter_context(SBT(nc, n, s, d))
        x32 = [sb("x32f%d" % i, [128, 16, 512], F32) for i in range(2)]; bx = [[Buf() for _ in range(16)] for _ in range(2)]
        ftAs = [sb("ftkA%d" % i, [128, 4, 2048], BF16) for i in range(2)]; ftBs = [sb("ftkB%d" % i, [128, 4, 2048], BF16) for i in range(2)]
        bfAs = [Buf(), Buf()]; bfBs = [Buf(), Buf()]
        zsq = [sb("zsqf%d" % i, [128, 512], F32) for i in range(2)]; bzsq = [Buf(), Buf()]
        mean = sb("meanf", [128, 512], F32); msq = sb("msqf", [128, 512], F32); var = sb("varf", [128, 512], F32); rstd = sb("rstdf", [128, 512], F32)
        bst = Buf()
        def stage1(tt):
            lat = tt < 8
            j = tt // 4 if lat else 2
            cols = slice(tt * 512, (tt + 1) * 512)
            i = tt % 2
            for hk in range(2):
                K.dma("sp", x32[i][:, hk * 8:(hk + 1) * 8, :], c.xs[hk * 1024:(hk + 1) * 1024, cols].rearrange("(c p) t -> p c t", p=128), [c.bxs[tt]], bx[i][hk * 8:(hk + 1) * 8])
            ftA, ftB, bfA, bfB = ftAs[i], ftBs[i], bfAs[i], bfBs[i]
            for tb in range(4):
                for (POS_, ft_, bf_) in ((c.POSA, ftA, bfA), (c.POSB, ftB, bfB)):
                    idx = POS_[:, tt * 4 + tb:tt * 4 + tb + 1]
                    dst = ft_[:, tb, :]
                    S.op("pool", lambda e, idx=idx, dst=dst: e.indirect_dma_start(out=dst, out_offset=None, in_=c.fsorted[:, :],
                                                                                  in_offset=bass.IndirectOffsetOnAxis(ap=idx, axis=0)),
                         [c.bfs, c.bPOS], [bf_], dma=True, part=True)
            for k in range(16):
                p_, bp_ = PP.next(0, 6)
                for tb in range(4):
                    K.mm(p_[:, tb * 128:(tb + 1) * 128], ftA[:, tb, k * 128:(k + 1) * 128], c.idb[:], True, False, [bfA, c.bc2], [bp_], part=True)
                    K.mm(p_[:, tb * 128:(tb + 1) * 128], ftB[:, tb, k * 128:(k + 1) * 128], c.idb[:], False, True, [bfB, c.bc2], [bp_], part=True)
                K.stt(x32[i][:, k, :], p_[:], c.modT[:, l, 80 + k, j:j + 1], x32[i][:, k, :], ALU.mult, ALU.add, [bp_, bx[i][k], c.bmod], [bx[i][k]])

        def stage2(tt):
            cols = slice(tt * 512, (tt + 1) * 512)
            i = tt % 2

            def post(k, i=i):
                K.act(x32[i][:, k, :], x32[i][:, k, :], AF.Identity, [bx[i][k], c.bconst], [bx[i][k]], bias=c.lnt[:, l, 3, k:k + 1], scale=c.lnt[:, l, 2, k:k + 1])
            ln_tile(c, K, x32[i], bx[i], None, None, LN_EPS, zsq, bzsq, mean, msq, var, rstd, bst, post)
            if l == 0:
                K.dma("sp", c.xs[:, cols].rearrange("(c p) t -> p c t", p=128), x32[i][:], bx[i], [c.bxs[tt]])
            else:
                K.dma("sp", c.out[:, cols].rearrange("(c p) t -> p c t", p=128), x32[i][:], bx[i], [c.bout], part=True)
        stage1(0)
        for tt in range(ntile):
            if tt + 1 < ntile:
                stage1(tt + 1)
            stage2(tt)
        S.barrier()


def _host_prep(inp):
    f = lambda a: np.ascontiguousarray(np.asarray(a, dtype=np.float32))
    sh = {}
    sh["w_ada"] = f(inp["w_ada"])
    sh["bada"] = f(np.asarray(inp["b_ada"]).reshape(2, 96, 128).transpose(2, 0, 1))
    ln = np.stack([np.asarray(inp["ln1_g"]), np.asarray(inp["ln1_b"]), np.asarray(inp["ln2_g"]), np.asarray(inp["ln2_b"])], 1)
    sh["lnp"] = f(ln.reshape(2, 4, 16, 128).transpose(3, 0, 1, 2))
    sh["w_in0"] = f(np.asarray(inp["w_in_even"])[0]); sh["w_in1"] = f(np.asarray(inp["w_in_odd"])[0])
    sh["w_out0"] = f(np.asarray(inp["w_out_even"])[0]); sh["w_out1"] = f(np.asarray(inp["w_out_odd"])[0])
    sh["sinkb"] = f(np.broadcast_to(np.asarray(inp["sink_logits"])[0][None, :], (128, 8)))
    rpb = np.asarray(inp["na_rpb"])[0]
    col = np.arange(64)
    dcol = np.clip(col[None, :] - col[:, None] + 15, 0, 30)
    sh["rpbx"] = f(rpb[:, :, dcol].transpose(3, 0, 1, 2))
    cs = np.clip(col - 8, 0, 48)
    col_ok = (col[None, :] >= cs[:, None]) & (col[None, :] < cs[:, None] + 16)
    sh["cmask"] = f(col_ok.T.astype(np.float32))
    sh["qkg"] = f(np.stack([np.asarray(inp["q_norm_g"])[0], np.asarray(inp["k_norm_g"])[0]], 1))
    sh["lamv"] = f(np.stack([np.asarray(inp[k])[0] for k in ("lambda_q1", "lambda_k1", "lambda_q2", "lambda_k2")], 1))
    sh["subln"] = f(np.asarray(inp["subln_g"])[0].reshape(2, 128).T)
    sh["wr"] = f(np.asarray(inp["w_router"]).reshape(16, 128, 16).transpose(1, 0, 2))
    sh["rbias"] = f(np.broadcast_to(np.tile(np.asarray(inp["router_bias"]), 4)[None, :], (128, 64)))
    sh["wg"] = f(inp["w_exp_gate"]).reshape(32, D, 1024); sh["wu"] = f(inp["w_exp_up"]).reshape(32, D, 1024); sh["wd"] = f(inp["w_exp_down"]).reshape(32, 1024, D)
    t = np.arange(2048)
    row = (t // 64).astype(np.float32); colt = (t % 64).astype(np.float32)
    inv = (np.float32(10000.0) ** (-np.arange(32, dtype=np.float32) / np.float32(32))).astype(np.float32)
    ang = np.concatenate([row[:, None] * inv[None], colt[:, None] * inv[None]], -1).astype(np.float32)
    cosv = np.cos(ang).astype(np.float32); sinv = np.sin(ang).astype(np.float32)
    sh["ropec"] = f(np.repeat(cosv.T, 2, axis=0)); sh["ropes"] = f(np.repeat(sinv.T, 2, axis=0))
    pm = np.zeros((128, 128), np.float32)
    for i in range(64):
        pm[2 * i + 1, 2 * i] = -1.0
        pm[2 * i, 2 * i + 1] = 1.0
    sh["perm"] = pm
    sh["ident"] = np.eye(128, dtype=np.float32)
    a = np.arange(128)
    tr = np.zeros((128, 2, 4, 128), np.float32)
    tr[:, 0] = (a[None, :] <= a[:, None]).astype(np.float32)[:, None, :]
    tr[:, 1] = (a[:, None] <= a[None, :]).astype(np.float32)[:, None, :]
    sh["trim"] = tr
    sh["ltri"] = (a[:, None] < a[None, :]).astype(np.float32)
    tg = np.zeros((128, 48), np.float32)
    for cc in range(16):
        for hh in range(2):
            tg[:, cc * 2 + hh] = 2 * (cc * 128 + a) + hh
    for jj in range(8):
        tg[:, 32 + jj] = jj * 128 + a
    sh["tgd"] = tg
    x = np.asarray(inp["x"]); ctx = np.asarray(inp["ctx"]); cc = np.asarray(inp["c"]); c_ctx = np.asarray(inp["c_ctx"])
    maps = []
    for i in range(NCORES):
        m = dict(sh)
        m["xin"] = f(np.concatenate([x[2 * i].T, x[2 * i + 1].T, ctx[2 * i].T, ctx[2 * i + 1].T], axis=1))
        m["cin"] = f(np.stack([cc[2 * i], cc[2 * i + 1], c_ctx], 0).reshape(3, 16, 128).transpose(2, 1, 0))
        maps.append(m)
    return maps


_NC_CACHE = {}


def kernel(**inputs):
    maps = _host_prep(inputs)
    if "nc" not in _NC_CACHE:
        _NC_CACHE["nc"] = build()
    nc = _NC_CACHE["nc"]
    res = run_bass_kernel_spmd(nc, maps, core_ids=list(range(NCORES)))
    y = np.empty((16, 2048, 2048), np.float32)
    for i in range(NCORES):
        o = np.asarray(res.results[i]["out"])
        y[2 * i] = o[:, 0:2048].T
        y[2 * i + 1] = o[:, 2048:4096].T
    return y
```
